# Optimizing a Trainium2 kernel written in Bass

```python
import jax, jax.numpy as jnp
from jax import lax
import numpy as np

D_MODEL = 2048
BATCH = 1
SEQ = 8192
DEPTH = 4

N_MIXERS = 2
POOL_WINDOWS = (2, 4, 8, 16)
N_POOL_GROUPS = 4
POOL_GROUP = D_MODEL // N_POOL_GROUPS
CONV_WIDTH = 31
CONV_PAD = CONV_WIDTH // 2
N_EXPERTS = 32
TOP_K = 4
D_EXPERT = D_MODEL // 2
SWIGLU_LIMIT = 7.0
SWIGLU_ALPHA = 1.702
LN_EPS = 1e-5
DEEPNORM_ALPHA = (2.0 * DEPTH) ** 0.25
DEEPNORM_BETA = (8.0 * DEPTH) ** -0.25
N_POOL_LAYERS = (DEPTH + 1) // 2
N_CONV_LAYERS = DEPTH // 2

kernel_name = "hybrid_pool_conformer_moe_deepnorm"


def layer_norm(x, g, b):
    xf = x.astype(jnp.float32)
    mu = jnp.mean(xf, axis=-1, keepdims=True)
    xc = xf - mu
    var = jnp.mean(xc * xc, axis=-1, keepdims=True)
    return (xc * lax.rsqrt(var + LN_EPS)).astype(x.dtype) * g + b


def pool_mixer(x, w, scale):
    B, S, D = x.shape
    xf = x.astype(jnp.float32).reshape(B, S, N_POOL_GROUPS, POOL_GROUP)
    cs = jnp.concatenate([jnp.zeros((B, 1, N_POOL_GROUPS, POOL_GROUP), jnp.float32),
                          jnp.cumsum(xf, axis=1)], axis=1)
    pos = jnp.arange(S)
    means = []
    for g, win in enumerate(POOL_WINDOWS):
        lo = jnp.clip(pos - win // 2, 0, S)
        hi = jnp.clip(pos + win // 2, 0, S)
        csg = cs[:, :, g]
        wsum = jnp.take(csg, hi, axis=1) - jnp.take(csg, lo, axis=1)
        cnt = (hi - lo).astype(jnp.float32)[None, :, None]
        means.append(wsum / cnt)
    pooled = jnp.stack(means, axis=2) - xf
    y = jnp.einsum('bsgc,gcd->bsgd', pooled.astype(x.dtype), w)
    return y.reshape(B, S, D) * scale


def conv_mixer(x, w1, b1, wdw, bdw, ln_g, ln_b, w2, b2):
    D = x.shape[-1]
    h = x @ w1 + b1
    a, gate = jnp.split(h, 2, axis=-1)
    h = a * jax.nn.sigmoid(gate)
    h = lax.conv_general_dilated(h, wdw[:, None, :], window_strides=(1,),
                                 padding=[(CONV_PAD, CONV_PAD)],
                                 dimension_numbers=('NWC', 'WIO', 'NWC'),
                                 feature_group_count=D) + bdw
    h = jax.nn.silu(layer_norm(h, ln_g, ln_b))
    return h @ w2 + b2


def moe_ffn(x, w_r, b_r, w1, b1, w2, b2):
    B, S, D = x.shape
    T = B * S
    xt = x.reshape(T, D)
    logits = (xt @ w_r + b_r).astype(jnp.float32)
    top_vals, top_idx = lax.top_k(logits, TOP_K)
    gates = jax.nn.softmax(top_vals, axis=-1).astype(x.dtype)
    flat_e = top_idx.reshape(-1)
    order = jnp.argsort(flat_e)
    e_sorted = flat_e[order]
    tok = order // TOP_K
    group_sizes = jnp.bincount(flat_e, length=N_EXPERTS).astype(jnp.int32)
    xs = xt[tok]
    h = lax.ragged_dot(xs, w1, group_sizes) + b1[e_sorted]
    glu = jnp.minimum(h[:, :D_EXPERT], SWIGLU_LIMIT)
    lin = jnp.clip(h[:, D_EXPERT:], -SWIGLU_LIMIT, SWIGLU_LIMIT)
    act = glu * jax.nn.sigmoid(SWIGLU_ALPHA * glu) * (lin + 1.0)
    y = lax.ragged_dot(act, w2, group_sizes) + b2[e_sorted]
    y = y * gates.reshape(-1)[order][:, None]
    out = jax.ops.segment_sum(y, tok, num_segments=T)
    return out.reshape(B, S, D)


def setup_inputs(seed: int = 0) -> dict:
    key = jax.random.key(seed)
    ks = jax.random.split(key, 24)
    D, C, F, E = D_MODEL, POOL_GROUP, D_EXPERT, N_EXPERTS
    nrm = jax.random.normal
    f32 = jnp.float32
    return {
        "x": nrm(ks[0], (BATCH, SEQ, D), f32),
        "pool_w": nrm(ks[1], (N_POOL_LAYERS, N_POOL_GROUPS, C, C), f32) * (C ** -0.5) * DEEPNORM_BETA,
        "pool_scale": 1.0 + 0.02 * nrm(ks[2], (N_POOL_LAYERS, D), f32),
        "conv_w1": nrm(ks[3], (N_CONV_LAYERS, D, 2 * D), f32) * (D ** -0.5),
        "conv_b1": 0.02 * nrm(ks[4], (N_CONV_LAYERS, 2 * D), f32),
        "conv_wdw": nrm(ks[5], (N_CONV_LAYERS, CONV_WIDTH, D), f32) * (CONV_WIDTH ** -0.5),
        "conv_bdw": 0.02 * nrm(ks[6], (N_CONV_LAYERS, D), f32),
        "conv_ln_g": 1.0 + 0.02 * nrm(ks[7], (N_CONV_LAYERS, D), f32),
        "conv_ln_b": 0.02 * nrm(ks[8], (N_CONV_LAYERS, D), f32),
        "conv_w2": nrm(ks[9], (N_CONV_LAYERS, D, D), f32) * (D ** -0.5) * DEEPNORM_BETA,
        "conv_b2": 0.02 * nrm(ks[10], (N_CONV_LAYERS, D), f32),
        "mix_ln_g": 1.0 + 0.02 * nrm(ks[11], (DEPTH, D), f32),
        "mix_ln_b": 0.02 * nrm(ks[12], (DEPTH, D), f32),
        "router_w": nrm(ks[13], (DEPTH, D, E), f32) * (D ** -0.5),
        "router_b": 0.01 * nrm(ks[14], (DEPTH, E), f32),
        "moe_w1": nrm(ks[15], (DEPTH, E, D, 2 * F), f32) * (D ** -0.5),
        "moe_b1": 0.02 * nrm(ks[16], (DEPTH, E, 2 * F), f32),
        "moe_w2": nrm(ks[17], (DEPTH, E, F, D), f32) * (F ** -0.5) * DEEPNORM_BETA,
        "moe_b2": 0.02 * nrm(ks[18], (DEPTH, E, D), f32),
        "ffn_ln_g": 1.0 + 0.02 * nrm(ks[19], (DEPTH, D), f32),
        "ffn_ln_b": 0.02 * nrm(ks[20], (DEPTH, D), f32),
    }


def reference(x, pool_w, pool_scale, conv_w1, conv_b1, conv_wdw, conv_bdw, conv_ln_g,
              conv_ln_b, conv_w2, conv_b2, mix_ln_g, mix_ln_b, router_w, router_b,
              moe_w1, moe_b1, moe_w2, moe_b2, ffn_ln_g, ffn_ln_b):
    for i in range(DEPTH):
        j = i // N_MIXERS
        if i % N_MIXERS == 0:
            mix = pool_mixer(x, pool_w[j], pool_scale[j])
        else:
            mix = conv_mixer(x, conv_w1[j], conv_b1[j], conv_wdw[j], conv_bdw[j],
                             conv_ln_g[j], conv_ln_b[j], conv_w2[j], conv_b2[j])
        x = layer_norm(DEEPNORM_ALPHA * x + mix, mix_ln_g[i], mix_ln_b[i])
        ffn = moe_ffn(x, router_w[i], router_b[i], moe_w1[i], moe_b1[i], moe_w2[i], moe_b2[i])
        x = layer_norm(DEEPNORM_ALPHA * x + ffn, ffn_ln_g[i], ffn_ln_b[i])
    return x
```

```python
import numpy as np
import ml_dtypes
from contextlib import ExitStack
import concourse.bass as bass
import concourse.mybir as mybir
from concourse.bass_utils import run_bass_kernel_spmd

F32 = mybir.dt.float32
BF16 = mybir.dt.bfloat16
ALU = mybir.AluOpType
AF = mybir.ActivationFunctionType

D = 2048
S = 8192
NCORES = 8
TOWN = S // NCORES
HALO = 64
T = TOWN + 2 * HALO
NT = T // 128
E = 32
TOPK = 4
F = 1024
NE = E // NCORES
DEPTH = 4
ALPHA = (2.0 * DEPTH) ** 0.25
LN_EPS = 1e-5
CONV_W = 31
CONV_PAD = 15
POOL_WINDOWS = (2, 4, 8, 16)


class _Eng:
    def __init__(self, name, sem):
        self.name = name
        self.sem = sem
        self.count = 0
        self.q = []
        self.waited = {}


class Prog:
    ENGS = ("sync", "scalar", "vector", "gpsimd", "tensor")

    def __init__(self, nc, es):
        self.nc = nc
        self.es = es
        self.eng = {n: _Eng(n, es.enter_context(nc.semaphore("po_" + n))) for n in self.ENGS}
        self.nsem = 0

    def dma_sem(self, name):
        self.nsem += 1
        return [self.es.enter_context(self.nc.semaphore(f"d_{name}_{self.nsem}")), 0]

    def _waits(self, e, deps):
        for d in deps:
            if d is None:
                continue
            s, v = d
            key = id(s)
            if e.waited.get(key, 0) < v:
                e.waited[key] = v
                e.q.append(lambda g, s=s, v=v: g.wait_ge(s, v))

    def op(self, eng, fn, deps=(), inc=True):
        e = self.eng[eng]
        self._waits(e, deps)
        if inc:
            e.count += 1
            sem = e.sem
            e.q.append(lambda g, fn=fn, sem=sem: fn(g).then_inc(sem, 1))
            return (e.sem, e.count)
        e.q.append(lambda g, fn=fn: fn(g))
        return None

    def dma(self, eng, out, in_, slot, deps=()):
        e = self.eng[eng]
        self._waits(e, deps)
        slot[1] += 16
        sem = slot[0]
        e.q.append(lambda g, out=out, in_=in_, sem=sem: g.dma_start(out=out, in_=in_).then_inc(sem, 16))
        return (sem, slot[1])

    def wait(self, eng, deps):
        self._waits(self.eng[eng], deps)

    def run(self):
        nc = self.nc
        with nc.Block() as block:
            for name in self.ENGS:
                q = self.eng[name].q

                def body(g, q=q):
                    for fn in q:
                        fn(g)
                getattr(block, name)(body)


class Bump:
    def __init__(self, big, nbytes):
        self.big = big
        self.nbytes = nbytes
        self.off = 0

    def alloc(self, shape_free, dtype):
        esz = 4 if dtype == F32 else 2
        n = int(np.prod(shape_free))
        nb = (n * esz + 31) // 32 * 32
        assert self.off + nb <= self.nbytes, f"SBUF overflow {self.off + nb} > {self.nbytes}"
        v = self.big[:, self.off // 4:(self.off + nb) // 4]
        self.off += nb
        if dtype != F32:
            v = v.bitcast(dtype)
        v = v[:, 0:n]
        if len(shape_free) == 2:
            v = v.rearrange("p (a b) -> p a b", a=shape_free[0])
        elif len(shape_free) == 3:
            v = v.rearrange("p (a b c) -> p a b c", a=shape_free[0], b=shape_free[1])
        return v

    def mark(self):
        return self.off

    def reset(self, m):
        self.off = m


SBUF_BYTES = 176 * 1024


def _blocks(C):
    nt = C // 128
    nb = (C + 511) // 512
    while nt % nb:
        nb += 1
    w = C // nb
    return [(i * w, w) for i in range(nb)]


def build_B(C, ne=NE, dbg=0):
    nc = bass.Bass("TRN2", target_bir_lowering=False)
    xsT = nc.dram_tensor("xsT", [ne, D, C], F32, kind="ExternalInput").ap()
    w1 = nc.dram_tensor("w1", [ne, D, 2 * F], F32, kind="ExternalInput").ap()
    b1t = nc.dram_tensor("b1t", [128, ne, 16], F32, kind="ExternalInput").ap()
    w2 = nc.dram_tensor("w2", [ne, F, D], F32, kind="ExternalInput").ap()
    b2t = nc.dram_tensor("b2t", [128, ne, 16], F32, kind="ExternalInput").ap()
    yT = nc.dram_tensor("yT", [ne, D, C], F32, kind="ExternalOutput").ap()
    blocks = _blocks(C)
    W = blocks[0][1]
    with ExitStack() as es:
        big = es.enter_context(nc.sbuf_tensor("big", [128, SBUF_BYTES // 4], F32))
        ps = [es.enter_context(nc.psum_tensor(f"ps{i}", [128, 512], F32)) for i in range(8)]
        P = Prog(nc, es)
        A = Bump(big, SBUF_BYTES)
        xs = A.alloc([16, C], BF16)
        act = A.alloc([8, C], BF16)
        NW1 = 3
        w1u = [A.alloc([16, 2, 256], BF16) for _ in range(NW1)]
        NW2 = 3
        w2u = [A.alloc([8, 512], BF16) for _ in range(NW2)]
        b1s = A.alloc([ne, 16], F32)
        b2s = A.alloc([ne, 16], F32)
        NTMP = 2
        gl = [A.alloc([W], F32) for _ in range(NTMP)]
        sg = [A.alloc([W], F32) for _ in range(NTMP)]
        lb = [A.alloc([W], F32) for _ in range(NTMP)]
        tt = [A.alloc([W], F32) for _ in range(NTMP)]
        NYS = 2
        ys = [A.alloc([C], F32) for _ in range(NYS)]

        s_b = P.dma_sem("b")
        t_b1 = P.dma("sync", b1s, b1t, s_b)
        t_b2 = P.dma("sync", b2s, b2t, s_b)
        t_bias = t_b2
        s_xs = P.dma_sem("xs")
        s_w1 = [P.dma_sem("w1") for _ in range(NW1)]
        s_w2 = [P.dma_sem("w2") for _ in range(NW2)]
        s_y = [P.dma_sem("y") for _ in range(NYS)]

        w1_free = [None] * NW1
        w2_free = [None] * NW2
        xs_free = None
        tmp_free = [None] * NTMP
        ys_dma = [None] * NYS
        act_free = None
        nw1 = 0
        nw2 = 0
        ntmp = 0
        nys = 0
        psw = 0
        psy = 0
        ps_free = [None] * 8
        out_tokens = []

        def load_xs(e):
            nonlocal xs_free
            return P.dma("gpsimd", xs, xsT[e].rearrange("(k p) c -> p k c", p=128), s_xs, deps=[xs_free])

        def load_w1(e, u):
            nonlocal nw1
            slot = nw1 % NW1
            nw1 += 1
            src = w1[e].rearrange("(k p) n -> p k n", p=128)
            P.dma("gpsimd", w1u[slot][:, :, 0, :], src[:, :, 256 * u:256 * (u + 1)], s_w1[slot], deps=[w1_free[slot]])
            tok = P.dma("gpsimd", w1u[slot][:, :, 1, :], src[:, :, F + 256 * u:F + 256 * (u + 1)], s_w1[slot])
            return slot, tok

        def load_w2(e, u):
            nonlocal nw2
            slot = nw2 % NW2
            nw2 += 1
            src = w2[e].rearrange("(k p) n -> p k n", p=128)[:, :, 512 * u:512 * (u + 1)]
            tok = P.dma("gpsimd", w2u[slot], src, s_w2[slot], deps=[w2_free[slot]])
            return slot, tok

        plan = []
        for e in range(ne):
            for u in range(4):
                plan.append(("w1", e, u))
            for u in range(4 if dbg != 1 else 0):
                plan.append(("w2", e, u))
        issued = {}
        nissued = 0

        def ensure(idx):
            nonlocal nissued
            while nissued <= idx and nissued < len(plan):
                kind, e, u = plan[nissued]
                issued[nissued] = load_w1(e, u) if kind == "w1" else load_w2(e, u)
                nissued += 1

        t_xs = load_xs(0)
        pi = 0
        for e in range(ne):
            for u in range(4):
                ensure(pi + 2)
                slot, t_w = issued[pi]
                pi += 1
                last_mm = None
                for fcl in range(2):
                    fc = 2 * u + fcl
                    for (c0, cw) in blocks:
                        bA = 2 * (psw % 2)
                        bB = bA + 1
                        psw += 1
                        pA, pB = ps[bA], ps[bB]
                        for k in range(16):
                            last = P.op("tensor", lambda g, k=k, pA=pA, slot=slot, fcl=fcl, c0=c0, cw=cw: g.matmul(
                                pA[:, 0:cw], lhsT=w1u[slot][:, k, 0, 128 * fcl:128 * (fcl + 1)], rhs=xs[:, k, c0:c0 + cw],
                                start=(k == 0), stop=(k == 15)),
                                deps=[t_w, t_xs, ps_free[bA], act_free] if k == 0 else (), inc=(k == 15))
                        t_A = last
                        for k in range(16):
                            last = P.op("tensor", lambda g, k=k, pB=pB, slot=slot, fcl=fcl, c0=c0, cw=cw: g.matmul(
                                pB[:, 0:cw], lhsT=w1u[slot][:, k, 1, 128 * fcl:128 * (fcl + 1)], rhs=xs[:, k, c0:c0 + cw],
                                start=(k == 0), stop=(k == 15)),
                                deps=[ps_free[bB]] if k == 0 else (), inc=(k == 15))
                        t_B = last
                        last_mm = t_B
                        ti = ntmp % NTMP
                        ntmp += 1
                        t1 = P.op("vector", lambda g, pA=pA, ti=ti, e=e, fc=fc, cw=cw: g.tensor_scalar(
                            out=gl[ti][:, 0:cw], in0=pA[:, 0:cw], scalar1=b1s[:, e, fc:fc + 1], scalar2=7.0,
                            op0=ALU.add, op1=ALU.min), deps=[t_A, t_bias, tmp_free[ti]])
                        ps_free[bA] = t1
                        t2 = P.op("scalar", lambda g, ti=ti, cw=cw: g.activation(
                            out=sg[ti][:, 0:cw], in_=gl[ti][:, 0:cw], func=AF.Sigmoid, scale=1.702), deps=[t1, tmp_free[ti]])
                        t3 = P.op("scalar", lambda g, pB=pB, ti=ti, e=e, fc=fc, cw=cw: g.activation(
                            out=lb[ti][:, 0:cw], in_=pB[:, 0:cw], func=AF.Identity, bias=b1s[:, e, 8 + fc:9 + fc], scale=1.0),
                            deps=[t_B, t_bias])
                        ps_free[bB] = t3
                        t4 = P.op("vector", lambda g, ti=ti, cw=cw: g.tensor_scalar(
                            out=lb[ti][:, 0:cw], in0=lb[ti][:, 0:cw], scalar1=7.0, scalar2=-7.0, op0=ALU.min, op1=ALU.max),
                            deps=[t3])
                        t5 = P.op("vector", lambda g, ti=ti, cw=cw: g.tensor_tensor(
                            out=tt[ti][:, 0:cw], in0=gl[ti][:, 0:cw], in1=sg[ti][:, 0:cw], op=ALU.mult), deps=[t2, t4])
                        t6 = P.op("vector", lambda g, ti=ti, fc=fc, c0=c0, cw=cw: g.scalar_tensor_tensor(
                            out=act[:, fc, c0:c0 + cw], in0=lb[ti][:, 0:cw], scalar=1.0, in1=tt[ti][:, 0:cw],
                            op0=ALU.add, op1=ALU.mult), deps=[t5])
                        tmp_free[ti] = t6
                        t_act = t6
                w1_free[slot] = last_mm
            xs_free = last_mm
            if e + 1 < ne:
                t_xs = load_xs(e + 1)
            for u in range(4 if dbg != 1 else 0):
                ensure(pi + 2)
                slot, t_w = issued[pi]
                pi += 1
                last_mm = None
                for dcl in range(4):
                    dc = 4 * u + dcl
                    yi = nys % NYS
                    nys += 1
                    t_ev = None
                    for (c0, cw) in blocks:
                        bY = 4 + (psy % 2)
                        psy += 1
                        pY = ps[bY]
                        for k in range(8):
                            last = P.op("tensor", lambda g, k=k, pY=pY, slot=slot, dcl=dcl, c0=c0, cw=cw: g.matmul(
                                pY[:, 0:cw], lhsT=w2u[slot][:, k, 128 * dcl:128 * (dcl + 1)], rhs=act[:, k, c0:c0 + cw],
                                start=(k == 0), stop=(k == 7)),
                                deps=[t_w, t_act, ps_free[bY]] if k == 0 else (), inc=(k == 7))
                        last_mm = last
                        t_ev = P.op("scalar", lambda g, pY=pY, yi=yi, e=e, dc=dc, c0=c0, cw=cw: g.activation(
                            out=ys[yi][:, c0:c0 + cw], in_=pY[:, 0:cw], func=AF.Identity, bias=b2s[:, e, dc:dc + 1], scale=1.0),
                            deps=[last, t_bias, ys_dma[yi]])
                        ps_free[bY] = t_ev
                    if dbg != 2:
                        ys_dma[yi] = P.dma("gpsimd", yT[e, 128 * dc:128 * (dc + 1), :], ys[yi], s_y[yi], deps=[t_ev])
                    else:
                        P.wait("sync", [t_ev])
                w2_free[slot] = last_mm
            act_free = last_mm
        P.wait("gpsimd", [ys_dma[i] for i in range(NYS)] + [t_act])
        P.run()
    return nc


class LNUnit:
    def __init__(self, P, A, g_bc, b_bc, t_params, nset=2):
        self.P = P
        self.g_bc, self.b_bc, self.t_params = g_bc, b_bc, t_params
        self.nset = nset
        self.st = [A.alloc([24], F32) for _ in range(nset)]
        self.mv = [A.alloc([2], F32) for _ in range(nset)]
        self.rstd = [A.alloc([1], F32) for _ in range(nset)]
        self.nmr = [A.alloc([1], F32) for _ in range(nset)]
        self.eps = A.alloc([1], F32)
        self.t_eps = P.op("vector", lambda g: g.memset(self.eps, LN_EPS))
        self.free = [None] * nset
        self.n = 0

    def emit(self, z, o, deps, o_free=None, gb=None):
        P = self.P
        i = self.n % self.nset
        self.n += 1
        st, mv, rstd, nmr = self.st[i], self.mv[i], self.rstd[i], self.nmr[i]
        g_bc, b_bc = gb if gb is not None else (self.g_bc, self.b_bc)
        t = None
        for c in range(4):
            t = P.op("vector", lambda g, c=c: g.bn_stats(out=st[:, 6 * c:6 * (c + 1)], in_=z[:, 512 * c:512 * (c + 1)]),
                     deps=list(deps) + [self.free[i]])
        t = P.op("vector", lambda g: g.bn_aggr(out=mv, in_=st), deps=[t])
        t = P.op("scalar", lambda g: g.activation(out=rstd, in_=mv[:, 1:2], func=AF.Sqrt, bias=self.eps[:, 0:1], scale=1.0),
                 deps=[t, self.t_eps])
        t = P.op("vector", lambda g: g.reciprocal(out=rstd, in_=rstd), deps=[t])
        t = P.op("vector", lambda g: g.tensor_scalar(out=nmr, in0=mv[:, 0:1], scalar1=rstd[:, 0:1], scalar2=-1.0,
                                                      op0=ALU.mult, op1=ALU.mult), deps=[t])
        t = P.op("scalar", lambda g: g.activation(out=o, in_=z, func=AF.Identity, scale=rstd[:, 0:1], bias=nmr[:, 0:1]),
                 deps=[t, o_free])
        self.free[i] = t
        t = P.op("gpsimd", lambda g: g.tensor_tensor(out=o, in0=o, in1=g_bc, op=ALU.mult), deps=[t, self.t_params])
        t = P.op("gpsimd", lambda g: g.tensor_tensor(out=o, in0=o, in1=b_bc, op=ALU.add), deps=[t])
        return t


def build_C(ntok=TOWN):
    nt = ntok // 128
    nc = bass.Bass("TRN2", target_bir_lowering=False)
    x1 = nc.dram_tensor("x1", [ntok, D], F32, kind="ExternalInput").ap()
    yk = nc.dram_tensor("yk", [ntok, TOPK, D], F32, kind="ExternalInput").ap()
    gk = nc.dram_tensor("gk", [ntok, TOPK], F32, kind="ExternalInput").ap()
    g_in = nc.dram_tensor("g_bc", [128, D], F32, kind="ExternalInput").ap()
    b_in = nc.dram_tensor("b_bc", [128, D], F32, kind="ExternalInput").ap()
    x2 = nc.dram_tensor("x2", [ntok, D], F32, kind="ExternalOutput").ap()
    with ExitStack() as es:
        big = es.enter_context(nc.sbuf_tensor("big", [128, SBUF_BYTES // 4], F32))
        P = Prog(nc, es)
        A = Bump(big, SBUF_BYTES)
        g_bc = A.alloc([D], F32)
        b_bc = A.alloc([D], F32)
        s_p = P.dma_sem("p")
        P.dma("sync", g_bc, g_in, s_p)
        t_params = P.dma("sync", b_bc, b_in, s_p)
        ln = LNUnit(P, A, g_bc, b_bc, t_params)
        NB = 2
        xin = [A.alloc([D], F32) for _ in range(NB)]
        yin = [A.alloc([TOPK, D], F32) for _ in range(NB)]
        gin = [A.alloc([TOPK], F32) for _ in range(NB)]
        ot = [A.alloc([D], F32) for _ in range(NB)]
        s_in = [P.dma_sem("in") for _ in range(NB)]
        s_out = [P.dma_sem("out") for _ in range(NB)]
        in_free = [None] * NB
        out_free = [None] * NB
        for j in range(nt):
            b = j % NB
            rows = slice(128 * j, 128 * (j + 1))
            P.dma("sync", xin[b], x1[rows, :], s_in[b], deps=[in_free[b]])
            P.dma("sync", gin[b], gk[rows, :], s_in[b])
            t_in = P.dma("sync", yin[b], yk[rows, :, :], s_in[b])
            t = P.op("scalar", lambda g, b=b: g.mul(xin[b], xin[b], ALPHA), deps=[t_in])
            for k in range(TOPK):
                t = P.op("vector", lambda g, b=b, k=k: g.scalar_tensor_tensor(
                    out=xin[b], in0=yin[b][:, k, :], scalar=gin[b][:, k:k + 1], in1=xin[b], op0=ALU.mult, op1=ALU.add),
                    deps=[t, t_in])
            t = ln.emit(xin[b], ot[b], [t], o_free=out_free[b])
            in_free[b] = t
            out_free[b] = P.dma("gpsimd", x2[rows, :], ot[b], s_out[b], deps=[t])
        P.wait("gpsimd", out_free)
        P.run()
    return nc


def pool_consts(core):
    tg = core * TOWN - HALO + np.arange(T)
    band = np.zeros((128, 4, NT, 3, 128), np.float32)
    invc = np.zeros((128, 4, T), np.float32)
    for g, w in enumerate(POOL_WINDOWS):
        lo = np.clip(tg - w // 2, 0, S)
        hi = np.clip(tg + w // 2, 0, S)
        cnt = np.maximum(hi - lo, 1)
        M = ((tg[:, None] >= lo[None, :]) & (tg[:, None] < hi[None, :])).astype(np.float32)
        M[np.arange(T), np.arange(T)] -= cnt
        invc[:, g, :] = (1.0 / cnt)[None, :]
        for j in range(NT):
            for r in range(3):
                jj = j + r - 1
                if 0 <= jj < NT:
                    band[:, g, j, r, :] = M[jj * 128:(jj + 1) * 128, j * 128:(j + 1) * 128]
    return band.reshape(128, 4, NT * 3, 128).astype(ml_dtypes.bfloat16), invc


def valid_mask(core):
    tg = core * TOWN - HALO + np.arange(T)
    return np.tile(((tg >= 0) & (tg < S)).astype(np.float32)[None, :], (128, 1))


def _emit_conv_phase(nc, P, A, ps, ps_free, ident, t_params, x, ins, stop=0):
    (w1_in, b1_in, wdw_in, bdw_in, lg_in, lb_in, w2_in, b2_in, mask_in) = ins
    TB = [(0, 384), (384, 384), (768, 384)]
    identb = A.alloc([128], BF16)
    ones = A.alloc([128], F32)
    cb1 = A.alloc([32], F32)
    wdw = A.alloc([16, CONV_W], F32)
    bdw = A.alloc([16], F32)
    clg = A.alloc([16], F32)
    clb = A.alloc([16], F32)
    eps = A.alloc([1], F32)
    s_c = P.dma_sem("cs")
    for dst, src in ((cb1, b1_in), (wdw, wdw_in), (bdw, bdw_in), (clg, lg_in)):
        P.dma("sync", dst, src, s_c)
    t_sm = P.dma("sync", clb, lb_in, s_c)
    P.op("vector", lambda g: g.memset(ones, 1.0))
    P.op("vector", lambda g: g.memset(eps, LN_EPS))
    t_idb = P.op("vector", lambda g: g.tensor_copy(out=identb, in_=ident), deps=[t_params])
    R1 = A.alloc([16, T], BF16)
    R2 = A.alloc([16, T], F32)
    m_R2 = A.mark() - 16 * T * 4
    m_R3 = A.mark()
    xT, uT, vT = R1, R1, R2
    xbf = A.alloc([NT, D], BF16)
    s_x = P.dma_sem("x")
    t_xbf = P.dma("gpsimd", xbf, x.rearrange("(j p) d -> p j d", p=128), s_x)
    n = 0
    t_a = None
    for j in range(NT):
        for h in range(2):
            bk = n % 2
            n += 1
            pb = ps[bk][:, :].bitcast(BF16)
            for i in range(8):
                k = 8 * h + i
                t_tr = P.op("tensor", lambda g, j=j, k=k, i=i, pb=pb: g.transpose(
                    out=pb[:, 128 * i:128 * (i + 1)], in_=xbf[:, j, 128 * k:128 * (k + 1)], identity=identb),
                    deps=[t_xbf, t_idb, ps_free[bk]] if i == 0 else (), inc=(i == 7))
            t_a = P.op("scalar" if n % 2 else "vector", lambda g, j=j, h=h, pb=pb: (g.copy if hasattr(g, "copy") else g.tensor_copy)(
                xT[:, 8 * h:8 * (h + 1), 128 * j:128 * (j + 1)], pb.rearrange("p (a c) -> p a c", a=8)), deps=[t_tr])
            ps_free[bk] = t_a
    t_a2 = P.op("vector", lambda g: g.memset(eps, LN_EPS), deps=[t_a, (P.eng["scalar"].sem, P.eng["scalar"].count)])
    t_a_done = t_a2
    if stop == 1:
        A.reset(m_R2)
        return t_a_done, None
    A.reset(m_R3)
    NW = 2
    w1u = [A.alloc([16, 2, 128], BF16) for _ in range(NW)]
    gpad = [A.alloc([T + 2 * CONV_PAD + 2], BF16) for _ in range(2)]
    Dg = [A.alloc([CONV_W, 128], BF16) for _ in range(2)]
    sgm = [A.alloc([384], F32) for _ in range(2)]
    mask = A.alloc([T], F32)
    s_m = P.dma_sem("m")
    t_mask = P.dma("sync", mask, mask_in, s_m, deps=[t_a_done])
    t_gz = None
    for gi in range(2):
        t_gz = P.op("vector", lambda g, gi=gi: g.memset(gpad[gi], 0.0), deps=[t_a_done])
    s_w1 = [P.dma_sem("cw1") for _ in range(NW)]
    w1_free = [t_a_done] * NW
    gpad_free = [t_gz] * 2
    Dg_free = [t_a_done] * 2
    sgm_free = [t_a_done] * 2
    w1v = w1_in.rearrange("(k p) n -> p k n", p=128)
    loads = {}

    def load_w1(i):
        sl = i % NW
        P.dma("gpsimd", w1u[sl][:, :, 0, :], w1v[:, :, 128 * i:128 * (i + 1)], s_w1[sl], deps=[w1_free[sl]])
        loads[i] = P.dma("gpsimd", w1u[sl][:, :, 1, :], w1v[:, :, D + 128 * i:D + 128 * (i + 1)], s_w1[sl])

    nW = 0
    nC = 0
    nsg = 0
    state = {}

    def emit_W(i):
        nonlocal nW, nsg
        sl = i % NW
        gi = i % 2
        di = i % 2
        t_d = None
        for k in range(CONV_W):
            t_d = P.op("vector", lambda g, di=di, k=k, i=i: g.tensor_scalar(
                out=Dg[di][:, k, :], in0=identb, scalar1=wdw[:, i, k:k + 1], scalar2=None, op0=ALU.mult),
                deps=[Dg_free[di], t_sm, t_idb] if k == 0 else ())
        t_gq = None
        t_mm = None
        for (c0, cw) in TB:
            bA = 2 + 2 * (nW % 2)
            bG = bA + 1
            nW += 1
            for k in range(16):
                t_A = P.op("tensor", lambda g, k=k, sl=sl, bA=bA, c0=c0, cw=cw: g.matmul(
                    ps[bA][:, 0:cw], lhsT=w1u[sl][:, k, 0, :], rhs=xT[:, k, c0:c0 + cw], start=(k == 0), stop=(k == 15)),
                    deps=[loads[i], t_a_done, ps_free[bA]] if k == 0 else (), inc=(k == 15))
            for k in range(16):
                t_G = P.op("tensor", lambda g, k=k, sl=sl, bG=bG, c0=c0, cw=cw: g.matmul(
                    ps[bG][:, 0:cw], lhsT=w1u[sl][:, k, 1, :], rhs=xT[:, k, c0:c0 + cw], start=(k == 0), stop=(k == 15)),
                    deps=[ps_free[bG]] if k == 0 else (), inc=(k == 15))
            t_mm = t_G
            si = nsg % 2
            nsg += 1
            t1 = P.op("scalar", lambda g, si=si, bG=bG, cw=cw, i=i: g.activation(
                out=sgm[si][:, 0:cw], in_=ps[bG][:, 0:cw], func=AF.Sigmoid, bias=cb1[:, 16 + i:17 + i], scale=1.0),
                deps=[t_G, t_sm, sgm_free[si]])
            ps_free[bG] = t1
            t2 = P.op("vector", lambda g, si=si, c0=c0, cw=cw: g.tensor_tensor(
                out=sgm[si][:, 0:cw], in0=sgm[si][:, 0:cw], in1=mask[:, c0:c0 + cw], op=ALU.mult), deps=[t1, t_mask])
            t_gq = P.op("vector", lambda g, si=si, gi=gi, bA=bA, c0=c0, cw=cw, i=i: g.scalar_tensor_tensor(
                out=gpad[gi][:, CONV_PAD + c0:CONV_PAD + c0 + cw], in0=ps[bA][:, 0:cw], scalar=cb1[:, i:i + 1],
                in1=sgm[si][:, 0:cw], op0=ALU.add, op1=ALU.mult), deps=[t2, t_A, gpad_free[gi]])
            ps_free[bA] = t_gq
            sgm_free[si] = t_gq
        w1_free[sl] = t_mm
        state[i] = (t_gq, t_d)

    t_v = None

    def emit_C(i):
        nonlocal nC, t_v
        gi = i % 2
        di = i % 2
        t_gq, t_d = state[i]
        t_mm = None
        for (c0, cw) in TB:
            bV = 6 + (nC % 2)
            nC += 1
            for k in range(CONV_W):
                t_mm = P.op("tensor", lambda g, k=k, di=di, gi=gi, bV=bV, c0=c0, cw=cw: g.matmul(
                    ps[bV][:, 0:cw], lhsT=Dg[di][:, k, :], rhs=gpad[gi][:, c0 + k:c0 + k + cw],
                    start=(k == 0), stop=(k == CONV_W - 1)),
                    deps=[t_gq, t_d, ps_free[bV]] if k == 0 else (), inc=(k == CONV_W - 1))
            t_v = P.op("scalar", lambda g, bV=bV, c0=c0, cw=cw, i=i: g.activation(
                out=vT[:, i, c0:c0 + cw], in_=ps[bV][:, 0:cw], func=AF.Identity, bias=bdw[:, i:i + 1], scale=1.0),
                deps=[t_mm, t_sm])
            ps_free[bV] = t_v
        gpad_free[gi] = t_mm
        Dg_free[di] = t_mm

    load_w1(0)
    for i in range(16):
        if i + 1 < 16:
            load_w1(i + 1)
        emit_W(i)
        if i >= 1:
            emit_C(i - 1)
    emit_C(15)
    t_bc_done = t_v
    if stop == 2:
        A.reset(m_R2)
        return t_bc_done, None
    A.reset(m_R3)
    mean = A.alloc([T], F32)
    rstd = A.alloc([T], F32)
    sq = [A.alloc([384], F32) for _ in range(2)]
    tmp = [A.alloc([T], F32) for _ in range(2)]
    sq_free = [t_bc_done] * 2
    t_st = None
    nsq = 0
    for (c0, cw) in TB:
        for i in range(16):
            t_s1 = P.op("tensor", lambda g, i=i, c0=c0, cw=cw: g.matmul(
                ps[0][:, 0:cw], lhsT=ones, rhs=vT[:, i, c0:c0 + cw], start=(i == 0), stop=(i == 15)),
                deps=[t_bc_done, ps_free[0]] if i == 0 else (), inc=(i == 15))
        for i in range(16):
            qi = nsq % 2
            nsq += 1
            t_q = P.op("scalar", lambda g, i=i, qi=qi, c0=c0, cw=cw: g.activation(
                out=sq[qi][:, 0:cw], in_=vT[:, i, c0:c0 + cw], func=AF.Square), deps=[t_bc_done, sq_free[qi]])
            t_s2 = P.op("tensor", lambda g, i=i, qi=qi, cw=cw: g.matmul(
                ps[1][:, 0:cw], lhsT=ones, rhs=sq[qi][:, 0:cw], start=(i == 0), stop=(i == 15)),
                deps=[t_q, ps_free[1]] if i == 0 else [t_q], inc=True)
            sq_free[qi] = t_s2
        t = P.op("vector", lambda g, c0=c0, cw=cw: g.tensor_scalar(
            out=mean[:, c0:c0 + cw], in0=ps[0][:, 0:cw], scalar1=1.0 / D, scalar2=None, op0=ALU.mult), deps=[t_s1, t_bc_done])
        ps_free[0] = t
        t = P.op("vector", lambda g, c0=c0, cw=cw: g.tensor_tensor(
            out=rstd[:, c0:c0 + cw], in0=mean[:, c0:c0 + cw], in1=mean[:, c0:c0 + cw], op=ALU.mult), deps=[t])
        t = P.op("vector", lambda g, c0=c0, cw=cw: g.scalar_tensor_tensor(
            out=rstd[:, c0:c0 + cw], in0=ps[1][:, 0:cw], scalar=1.0 / D, in1=rstd[:, c0:c0 + cw],
            op0=ALU.mult, op1=ALU.subtract), deps=[t, t_s2])
        ps_free[1] = t
        t = P.op("scalar", lambda g, c0=c0, cw=cw: g.activation(
            out=rstd[:, c0:c0 + cw], in_=rstd[:, c0:c0 + cw], func=AF.Sqrt, bias=eps[:, 0:1], scale=1.0), deps=[t])
        t_st = P.op("vector", lambda g, c0=c0, cw=cw: g.reciprocal(out=rstd[:, c0:c0 + cw], in_=rstd[:, c0:c0 + cw]), deps=[t])
    if stop == 3:
        A.reset(m_R2)
        return t_st, None
    tmp_free = [t_bc_done] * 2
    t_u = None
    for i in range(16):
        ti = i % 2
        t = P.op("vector", lambda g, i=i, ti=ti: g.tensor_tensor(out=tmp[ti], in0=vT[:, i, :], in1=mean, op=ALU.subtract),
                 deps=[t_st, tmp_free[ti]])
        t = P.op("gpsimd", lambda g, ti=ti: g.tensor_tensor(out=tmp[ti], in0=tmp[ti], in1=rstd, op=ALU.mult), deps=[t, t_st])
        t_u = P.op("scalar", lambda g, i=i, ti=ti: g.activation(
            out=uT[:, i, :], in_=tmp[ti], func=AF.Silu, scale=clg[:, i:i + 1], bias=clb[:, i:i + 1]), deps=[t, t_sm, t_bc_done])
        tmp_free[ti] = t_u
    t_d_done = P.op("vector", lambda g: g.memset(eps, LN_EPS), deps=[t_u])
    if stop == 4:
        A.reset(m_R2)
        return t_d_done, None
    A.reset(m_R3)
    NW2 = 2
    w2u = [A.alloc([16, 512], BF16) for _ in range(NW2)]
    cb2 = A.alloc([D], F32)
    s_w2 = [P.dma_sem("cw2") for _ in range(NW2)]
    s_b2 = P.dma_sem("cb2")
    t_b2 = P.dma("sync", cb2, b2_in, s_b2, deps=[t_d_done])
    w2_free = [t_d_done] * NW2
    w2v = w2_in.rearrange("(k p) n -> p k n", p=128)
    w2loads = {}
    nl = [0]

    def ensure(idx):
        while nl[0] <= idx and nl[0] < NT * 4:
            nb = nl[0] % 4
            sl = nl[0] % NW2
            w2loads[nl[0]] = P.dma("gpsimd", w2u[sl], w2v[:, :, 512 * nb:512 * (nb + 1)], s_w2[sl], deps=[w2_free[sl]])
            nl[0] += 1

    def tail(j, mixs_b, mix_free_b):
        t_m = None
        for nb in range(4):
            idx = 4 * j + nb
            ensure(idx + 1)
            sl = idx % NW2
            bk = 2 + nb
            for k in range(16):
                t_mm = P.op("tensor", lambda g, k=k, sl=sl, bk=bk, j=j: g.matmul(
                    ps[bk][:, :], lhsT=uT[:, k, 128 * j:128 * (j + 1)], rhs=w2u[sl][:, k, :], start=(k == 0), stop=(k == 15)),
                    deps=[w2loads[idx], t_d_done, ps_free[bk]] if k == 0 else (), inc=(k == 15))
            w2_free[sl] = t_mm
            t_m = P.op("vector", lambda g, nb=nb, bk=bk: g.tensor_tensor(
                out=mixs_b[:, 512 * nb:512 * (nb + 1)], in0=ps[bk][:, :], in1=cb2[:, 512 * nb:512 * (nb + 1)], op=ALU.add),
                deps=[t_mm, t_b2, mix_free_b])
            ps_free[bk] = t_m
        return t_m

    A.reset(m_R2)
    A.nbytes = m_R3
    return t_d_done, tail


def build_A(kind, stop=0):
    nc = bass.Bass("TRN2", target_bir_lowering=False)
    inp = lambda name, shape, dt=F32: nc.dram_tensor(name, shape, dt, kind="ExternalInput").ap()
    x = inp("x", [T, D])
    ident_in = inp("ident", [128, 128])
    mg_in = inp("mg_bc", [128, D])
    mb_in = inp("mb_bc", [128, D])
    wr_in = inp("wr", [D, E])
    br_in = inp("br_bc", [128, E])
    if kind == "pool":
        band_in = inp("band", [128, 4, NT * 3, 128], BF16)
        invc_in = inp("invc", [128, 4, T])
        wp_in = inp("wp", [4, 512, 512])
        sc_in = inp("sc_bc", [128, D])
    else:
        w1_in = inp("cw1", [D, 2 * D])
        b1_in = inp("cb1t", [128, 32])
        wdw_in = inp("wdwt", [128, 16, CONV_W])
        bdw_in = inp("bdwt", [128, 16])
        lg_in = inp("clgt", [128, 16])
        lb_in = inp("clbt", [128, 16])
        w2_in = inp("cw2", [D, D])
        b2_in = inp("cb2_bc", [128, D])
        mask_in = inp("mask", [128, T])
    x1o = nc.dram_tensor("x1", [T, D], F32, kind="ExternalOutput").ap()
    Go = nc.dram_tensor("G", [T, E], F32, kind="ExternalOutput").ap()
    with ExitStack() as es:
        big = es.enter_context(nc.sbuf_tensor("big", [128, SBUF_BYTES // 4], F32))
        ps = [es.enter_context(nc.psum_tensor(f"ps{i}", [128, 512], F32)) for i in range(8)]
        P = Prog(nc, es)
        A = Bump(big, SBUF_BYTES)
        ps_free = [None] * 8
        ident = A.alloc([128], F32)
        mg_bc = A.alloc([D], F32)
        mb_bc = A.alloc([D], F32)
        wr_sb = A.alloc([16, E], F32)
        br_bc = A.alloc([E], F32)
        s_p = P.dma_sem("p")
        P.dma("sync", ident, ident_in, s_p)
        P.dma("sync", mg_bc, mg_in, s_p)
        P.dma("sync", mb_bc, mb_in, s_p)
        P.dma("sync", wr_sb, wr_in.rearrange("(k p) e -> p k e", p=128), s_p)
        t_params = P.dma("sync", br_bc, br_in, s_p)
        ln = LNUnit(P, A, mg_bc, mb_bc, t_params)
        s_x = P.dma_sem("x")
        if kind == "pool":
            pooledT = A.alloc([16, T], BF16)
            wpb = A.alloc([4, 4, 512], BF16)
            sc_bc = A.alloc([D], F32)
        m_phase = A.mark()
        if kind == "pool":
            xbf = A.alloc([NT, D], BF16)
            t_xbf = P.dma("gpsimd", xbf, x.rearrange("(j p) d -> p j d", p=128), s_x)

        if kind == "pool":
            s_w = P.dma_sem("w")
            for g in range(4):
                t_wp = P.dma("gpsimd", wpb[:, g, :, :], wp_in[g].rearrange("(k p) n -> p k n", p=128), s_w)
            s_sc = P.dma_sem("sc")
            t_sc = P.dma("sync", sc_bc, sc_in, s_sc)
            band = A.alloc([4, NT * 3, 128], BF16)
            invc = A.alloc([4, T], F32)
            s_bi = P.dma_sem("bi")
            P.dma("sync", band, band_in, s_bi)
            t_bi = P.dma("sync", invc, invc_in, s_bi)
            nbank = 0
            t_ev = None
            for cc in range(16):
                g = cc // 4
                for jb in range(0, NT, 4):
                    js = list(range(jb, min(jb + 4, NT)))
                    bk = nbank % 2
                    nbank += 1
                    first = True
                    for j in js:
                        rels = [r for r in range(3) if 0 <= j + r - 1 < NT]
                        for ri, r in enumerate(rels):
                            last = (j == js[-1] and ri == len(rels) - 1)
                            t_mm = P.op("tensor", lambda gg, cc=cc, g=g, j=j, r=r, bk=bk, jb=jb, ri=ri, n=len(rels): gg.matmul(
                                ps[bk][:, 128 * (j - jb):128 * (j - jb + 1)], lhsT=xbf[:, j + r - 1, 128 * cc:128 * (cc + 1)],
                                rhs=band[:, g, 3 * j + r, :], start=(ri == 0), stop=(ri == n - 1)),
                                deps=[t_xbf, t_bi, ps_free[bk]] if first else (), inc=last)
                            first = False
                    w = 128 * len(js)
                    t_ev = P.op("vector", lambda gg, cc=cc, g=g, bk=bk, jb=jb, w=w: gg.tensor_tensor(
                        out=pooledT[:, cc, 128 * jb:128 * jb + w], in0=ps[bk][:, 0:w], in1=invc[:, g, 128 * jb:128 * jb + w],
                        op=ALU.mult), deps=[t_mm, t_bi])
                    ps_free[bk] = t_ev
            t_phase1 = t_ev
            A.reset(m_phase)
        else:
            t_phase1, conv = _emit_conv_phase(nc, P, A, ps, ps_free, ident, t_params, x,
                                              (w1_in, b1_in, wdw_in, bdw_in, lg_in, lb_in, w2_in, b2_in, mask_in), stop)

        NB = 2
        xt = [A.alloc([D], F32) for _ in range(NB)]
        mixs = [A.alloc([D], F32) for _ in range(NB)]
        x1t = [A.alloc([D], F32) for _ in range(NB)]
        x1T = [A.alloc([16, 128], F32) for _ in range(NB)]
        lg = [A.alloc([E], F32) for _ in range(NB)]
        ex = [A.alloc([E], F32) for _ in range(NB)]
        top8 = [A.alloc([8], F32) for _ in range(NB)]
        sm = [A.alloc([4], F32) for _ in range(NB)]
        Gt = [A.alloc([E], F32) for _ in range(NB)]
        s_xt = [P.dma_sem("xt") for _ in range(NB)]
        s_o = [P.dma_sem("o") for _ in range(NB)]
        s_g = [P.dma_sem("g") for _ in range(NB)]
        xt_free = [t_phase1] * NB
        mix_free = [t_phase1] * NB
        x1t_free = [t_phase1] * NB
        x1T_free = [t_phase1] * NB
        sm_free = [t_phase1] * NB
        G_free = [None] * NB
        nps = 0
        for j in range(NT if not stop else 0):
            b = j % NB
            rows = slice(128 * j, 128 * (j + 1))
            t_xt = P.dma("sync", xt[b], x[rows, :], s_xt[b], deps=[xt_free[b]])
            if kind == "pool":
                t_m = None
                for g in range(4):
                    bk = 2 + g
                    for kk in range(4):
                        t_mm = P.op("tensor", lambda gg, g=g, kk=kk, j=j, bk=bk: gg.matmul(
                            ps[bk][:, :], lhsT=pooledT[:, 4 * g + kk, 128 * j:128 * (j + 1)], rhs=wpb[:, g, kk, :],
                            start=(kk == 0), stop=(kk == 3)),
                            deps=[t_phase1, t_wp, ps_free[bk]] if kk == 0 else (), inc=(kk == 3))
                    t_m = P.op("vector", lambda gg, g=g, b=b, bk=bk: gg.tensor_tensor(
                        out=mixs[b][:, 512 * g:512 * (g + 1)], in0=ps[bk][:, :], in1=sc_bc[:, 512 * g:512 * (g + 1)], op=ALU.mult),
                        deps=[t_mm, t_sc, mix_free[b]])
                    ps_free[bk] = t_m
            else:
                t_m = conv(j, mixs[b], mix_free[b])
            t_z = P.op("vector", lambda gg, b=b: gg.scalar_tensor_tensor(
                out=mixs[b], in0=xt[b], scalar=ALPHA, in1=mixs[b], op0=ALU.mult, op1=ALU.add), deps=[t_m, t_xt])
            xt_free[b] = t_z
            t_x1 = ln.emit(mixs[b], x1t[b], [t_z], o_free=x1t_free[b])
            mix_free[b] = t_x1
            t_st = P.dma("gpsimd", x1o[rows, :], x1t[b], s_o[b], deps=[t_x1])
            t_cp = None
            for q in range(4):
                bk = 6 + (nps % 2)
                nps += 1
                for i in range(4):
                    k = 4 * q + i
                    t_tr = P.op("tensor", lambda gg, b=b, k=k, i=i, bk=bk: gg.transpose(
                        out=ps[bk][:, 128 * i:128 * (i + 1)], in_=x1t[b][:, 128 * k:128 * (k + 1)], identity=ident),
                        deps=[t_x1, t_params, ps_free[bk]] if i == 0 else (), inc=(i == 3))
                t_cp = P.op("scalar", lambda gg, b=b, q=q, bk=bk: gg.copy(
                    x1T[b][:, 4 * q:4 * (q + 1), :], ps[bk][:, :].rearrange("p (a c) -> p a c", a=4)),
                    deps=[t_tr, x1T_free[b]])
                ps_free[bk] = t_cp
            x1t_free[b] = (t_tr if t_st is None else t_tr)
            lbk = j % 2
            for k in range(16):
                t_lg = P.op("tensor", lambda gg, b=b, k=k, lbk=lbk: gg.matmul(
                    ps[lbk][:, 0:E], lhsT=x1T[b][:, k, :], rhs=wr_sb[:, k, :], start=(k == 0), stop=(k == 15)),
                    deps=[t_cp, ps_free[lbk], t_phase1] if k == 0 else (), inc=(k == 15))
            x1T_free[b] = t_lg
            t = P.op("vector", lambda gg, b=b, lbk=lbk: gg.tensor_tensor(out=lg[b], in0=ps[lbk][:, 0:E], in1=br_bc, op=ALU.add),
                     deps=[t_lg, t_params, sm_free[b], G_free[b]])
            ps_free[lbk] = t
            t = P.op("vector", lambda gg, b=b: gg.max(out=top8[b], in_=lg[b]), deps=[t])
            t = P.op("vector", lambda gg, b=b: gg.tensor_scalar(out=sm[b][:, 0:1], in0=top8[b][:, 0:1], scalar1=-1.0, scalar2=None,
                                                                 op0=ALU.mult), deps=[t])
            t = P.op("scalar", lambda gg, b=b: gg.activation(out=ex[b], in_=lg[b], func=AF.Exp, bias=sm[b][:, 0:1], scale=1.0),
                     deps=[t])
            t = P.op("vector", lambda gg, b=b: gg.scalar_tensor_tensor(
                out=ex[b], in0=lg[b], scalar=top8[b][:, 3:4], in1=ex[b], op0=ALU.is_ge, op1=ALU.mult), deps=[t])
            t = P.op("vector", lambda gg, b=b: gg.tensor_reduce(out=sm[b][:, 1:2], in_=ex[b], axis=mybir.AxisListType.X, op=ALU.add),
                     deps=[t])
            t = P.op("vector", lambda gg, b=b: gg.reciprocal(out=sm[b][:, 2:3], in_=sm[b][:, 1:2]), deps=[t])
            t = P.op("vector", lambda gg, b=b: gg.tensor_scalar(out=Gt[b], in0=ex[b], scalar1=sm[b][:, 2:3], scalar2=None,
                                                                 op0=ALU.mult), deps=[t])
            sm_free[b] = t
            G_free[b] = P.dma("gpsimd", Go[rows, :], Gt[b], s_g[b], deps=[t])
            x1t_free[b] = t_st
            P.wait("scalar", [t_tr])
        P.wait("gpsimd", [G_free[i] for i in range(NB)] + [(s_o[i][0], s_o[i][1]) for i in range(NB) if s_o[i][1]] + [t_phase1])
        P.run()
    return nc


def _bc(v):
    return np.ascontiguousarray(np.broadcast_to(np.asarray(v, np.float32)[None, :], (128, v.shape[0])))


def _slice_halo(xfull, core):
    out = np.zeros((T, D), np.float32)
    lo = core * TOWN - HALO
    a, b = max(lo, 0), min(lo + T, S)
    out[a - lo:b - lo] = xfull[a:b]
    return out


def _chunk_t(v, n):
    return np.ascontiguousarray(np.asarray(v, np.float32).reshape(n, 128).T)


_IDENT = np.eye(128, dtype=np.float32)
_POOLC = {}


def inputs_A_pool(core, xfull, pool_w, pool_scale, mg, mb, wr, br):
    if core not in _POOLC:
        _POOLC[core] = pool_consts(core)
    band, invc = _POOLC[core]
    return {"x": _slice_halo(xfull, core), "ident": _IDENT, "mg_bc": _bc(mg), "mb_bc": _bc(mb), "wr": np.ascontiguousarray(wr),
            "br_bc": _bc(br), "band": band, "invc": invc, "wp": np.ascontiguousarray(pool_w), "sc_bc": _bc(pool_scale)}


def inputs_A_conv(core, xfull, w1, b1, wdw, bdw, lng, lnb, w2, b2, mg, mb, wr, br):
    wdwt = np.ascontiguousarray(np.asarray(wdw, np.float32).T.reshape(16, 128, CONV_W).transpose(1, 0, 2))
    return {"x": _slice_halo(xfull, core), "ident": _IDENT, "mg_bc": _bc(mg), "mb_bc": _bc(mb), "wr": np.ascontiguousarray(wr),
            "br_bc": _bc(br), "cw1": np.ascontiguousarray(w1), "cb1t": _chunk_t(b1, 32), "wdwt": wdwt,
            "bdwt": _chunk_t(bdw, 16), "clgt": _chunk_t(lng, 16), "clbt": _chunk_t(lnb, 16),
            "cw2": np.ascontiguousarray(w2), "cb2_bc": _bc(b2), "mask": valid_mask(core)}


_PROGS = {}


def _prog(key, builder):
    if key not in _PROGS:
        _PROGS[key] = builder()
    return _PROGS[key]


def _run(nc, in_maps):
    res = run_bass_kernel_spmd(nc, in_maps, core_ids=list(range(NCORES)))
    return res.results


def _lay_bias(b):
    ne = b.shape[0]
    return np.ascontiguousarray(np.asarray(b, np.float32).reshape(ne, 16, 128).transpose(2, 0, 1))


def kernel(x, pool_w, pool_scale, conv_w1, conv_b1, conv_wdw, conv_bdw, conv_ln_g, conv_ln_b, conv_w2, conv_b2,
           mix_ln_g, mix_ln_b, router_w, router_b, moe_w1, moe_b1, moe_w2, moe_b2, ffn_ln_g, ffn_ln_b):
    f32 = lambda a: np.asarray(a, dtype=np.float32)
    xcur = np.ascontiguousarray(f32(x)[0])
    cen = slice(HALO, HALO + TOWN)
    for i in range(DEPTH):
        j = i // 2
        if i % 2 == 0:
            ncA = _prog("A_pool", lambda: build_A("pool"))
            maps = [inputs_A_pool(c, xcur, f32(pool_w[j]), f32(pool_scale[j]), f32(mix_ln_g[i]), f32(mix_ln_b[i]),
                                  f32(router_w[i]), f32(router_b[i])) for c in range(NCORES)]
        else:
            ncA = _prog("A_conv", lambda: build_A("conv"))
            maps = [inputs_A_conv(c, xcur, f32(conv_w1[j]), f32(conv_b1[j]), f32(conv_wdw[j]), f32(conv_bdw[j]),
                                  f32(conv_ln_g[j]), f32(conv_ln_b[j]), f32(conv_w2[j]), f32(conv_b2[j]),
                                  f32(mix_ln_g[i]), f32(mix_ln_b[i]), f32(router_w[i]), f32(router_b[i]))
                    for c in range(NCORES)]
        res = _run(ncA, maps)
        del maps
        x1 = np.concatenate([r["x1"][cen] for r in res], axis=0)
        G = np.concatenate([r["G"][cen] for r in res], axis=0)
        del res
        sel = G > 0
        toks = [np.nonzero(sel[:, e])[0] for e in range(E)]
        cmax = max(int(t.shape[0]) for t in toks)
        C = max(128, (cmax + 127) // 128 * 128)
        kpos = np.cumsum(sel, axis=1) - 1
        maps = []
        for c in range(NCORES):
            xsT = np.zeros((NE, D, C), np.float32)
            for m in range(NE):
                t = toks[c + NCORES * m]
                xsT[m, :, :t.shape[0]] = x1[t].T
            maps.append({"xsT": xsT, "w1": f32(moe_w1[i][c::NCORES]), "b1t": _lay_bias(f32(moe_b1[i][c::NCORES])),
                         "w2": f32(moe_w2[i][c::NCORES]), "b2t": _lay_bias(f32(moe_b2[i][c::NCORES]))})
        ncB = _prog(("B", C), lambda: build_B(C))
        res = _run(ncB, maps)
        del maps
        yk = np.zeros((S, TOPK, D), np.float32)
        gk = np.zeros((S, TOPK), np.float32)
        for c in range(NCORES):
            for m in range(NE):
                e = c + NCORES * m
                t = toks[e]
                kp = np.minimum(kpos[t, e], TOPK - 1)
                yk[t, kp, :] = res[c]["yT"][m][:, :t.shape[0]].T
                gk[t, kp] = G[t, e]
        del res
        ncC = _prog("C", lambda: build_C(TOWN))
        gb, bb = _bc(f32(ffn_ln_g[i])), _bc(f32(ffn_ln_b[i]))
        maps = [{"x1": x1[c * TOWN:(c + 1) * TOWN], "yk": yk[c * TOWN:(c + 1) * TOWN], "gk": gk[c * TOWN:(c + 1) * TOWN],
                 "g_bc": gb, "b_bc": bb} for c in range(NCORES)]
        res = _run(ncC, maps)
        del maps, yk
        xcur = np.concatenate([r["x2"] for r in res], axis=0)
        del res
    return np.ascontiguousarray(xcur[None].astype(np.float32))
```

```python
import numpy as np
import ml_dtypes
from contextlib import ExitStack
import concourse.bass as bass
import concourse.mybir as mybir
from concourse.bass_utils import run_bass_kernel_spmd

F32 = mybir.dt.float32
BF16 = mybir.dt.bfloat16
ALU = mybir.AluOpType
AF = mybir.ActivationFunctionType

D = 2048
S = 8192
NCORES = 8
TOWN = S // NCORES
HALO = 64
T = TOWN + 2 * HALO
NT = T // 128
E = 32
TOPK = 4
F = 1024
NE = E // NCORES
DEPTH = 4
ALPHA = (2.0 * DEPTH) ** 0.25
LN_EPS = 1e-5
CONV_W = 31
CONV_PAD = 15
POOL_WINDOWS = (2, 4, 8, 16)


class _Eng:
    def __init__(self, name, sem):
        self.name = name
        self.sem = sem
        self.count = 0
        self.q = []
        self.waited = {}


class Prog:
    ENGS = ("sync", "scalar", "vector", "gpsimd", "tensor")

    def __init__(self, nc, es):
        self.nc = nc
        self.es = es
        self.eng = {n: _Eng(n, es.enter_context(nc.semaphore("po_" + n))) for n in self.ENGS}
        self.nsem = 0

    def dma_sem(self, name):
        self.nsem += 1
        return [self.es.enter_context(self.nc.semaphore(f"d_{name}_{self.nsem}")), 0]

    def _waits(self, e, deps):
        for d in deps:
            if d is None:
                continue
            s, v = d
            key = id(s)
            if e.waited.get(key, 0) < v:
                e.waited[key] = v
                e.q.append(lambda g, s=s, v=v: g.wait_ge(s, v))

    def op(self, eng, fn, deps=(), inc=True):
        e = self.eng[eng]
        self._waits(e, deps)
        if inc:
            e.count += 1
            sem = e.sem
            e.q.append(lambda g, fn=fn, sem=sem: fn(g).then_inc(sem, 1))
            return (e.sem, e.count)
        e.q.append(lambda g, fn=fn: fn(g))
        return None

    def dma(self, eng, out, in_, slot, deps=()):
        e = self.eng[eng]
        self._waits(e, deps)
        slot[1] += 16
        sem = slot[0]
        e.q.append(lambda g, out=out, in_=in_, sem=sem: g.dma_start(out=out, in_=in_).then_inc(sem, 16))
        return (sem, slot[1])

    def wait(self, eng, deps):
        self._waits(self.eng[eng], deps)

    def run(self):
        nc = self.nc
        with nc.Block() as block:
            for name in self.ENGS:
                q = self.eng[name].q

                def body(g, q=q):
                    for fn in q:
                        fn(g)
                getattr(block, name)(body)


class Bump:
    def __init__(self, big, nbytes):
        self.big = big
        self.nbytes = nbytes
        self.off = 0

    def alloc(self, shape_free, dtype):
        esz = 4 if dtype == F32 else 2
        n = int(np.prod(shape_free))
        nb = (n * esz + 31) // 32 * 32
        assert self.off + nb <= self.nbytes, f"SBUF overflow {self.off + nb} > {self.nbytes}"
        v = self.big[:, self.off // 4:(self.off + nb) // 4]
        self.off += nb
        if dtype != F32:
            v = v.bitcast(dtype)
        v = v[:, 0:n]
        if len(shape_free) == 2:
            v = v.rearrange("p (a b) -> p a b", a=shape_free[0])
        elif len(shape_free) == 3:
            v = v.rearrange("p (a b c) -> p a b c", a=shape_free[0], b=shape_free[1])
        return v

    def mark(self):
        return self.off

    def reset(self, m):
        self.off = m


SBUF_BYTES = 176 * 1024


def _blocks(C):
    nt = C // 128
    nb = (C + 511) // 512
    if nt % nb == 0:
        w = C // nb
        return [(i * w, w) for i in range(nb)]
    out, c0 = [], 0
    while c0 < C:
        w = min(512, C - c0)
        out.append((c0, w))
        c0 += w
    return out


def build_B(C, ne=NE, dbg=0):
    nc = bass.Bass("TRN2", target_bir_lowering=False)
    xsT = nc.dram_tensor("xsT", [ne, D, C], F32, kind="ExternalInput").ap()
    w1 = nc.dram_tensor("w1", [ne, D, 2 * F], F32, kind="ExternalInput").ap()
    b1t = nc.dram_tensor("b1t", [128, ne, 16], F32, kind="ExternalInput").ap()
    w2 = nc.dram_tensor("w2", [ne, F, D], F32, kind="ExternalInput").ap()
    b2t = nc.dram_tensor("b2t", [128, ne, 16], F32, kind="ExternalInput").ap()
    yT = nc.dram_tensor("yT", [ne, D, C], F32, kind="ExternalOutput").ap()
    blocks = _blocks(C)
    W = max(w for _, w in blocks)
    with ExitStack() as es:
        big = es.enter_context(nc.sbuf_tensor("big", [128, SBUF_BYTES // 4], F32))
        ps = [es.enter_context(nc.psum_tensor(f"ps{i}", [128, 512], F32)) for i in range(8)]
        P = Prog(nc, es)
        A = Bump(big, SBUF_BYTES)
        xs = A.alloc([16, C], BF16)
        act = A.alloc([8, C], BF16)
        NW1 = 3
        w1u = [A.alloc([16, 2, 256], BF16) for _ in range(NW1)]
        NW2 = 3
        w2u = [A.alloc([8, 512], BF16) for _ in range(NW2)]
        b1s = A.alloc([ne, 16], F32)
        b2s = A.alloc([ne, 16], F32)
        NTMP = 2
        gl = [A.alloc([W], F32) for _ in range(NTMP)]
        sg = [A.alloc([W], F32) for _ in range(NTMP)]
        lb = [A.alloc([W], F32) for _ in range(NTMP)]
        tt = [A.alloc([W], F32) for _ in range(NTMP)]
        NYS = 2
        ys = [A.alloc([C], F32) for _ in range(NYS)]

        s_b = P.dma_sem("b")
        t_b1 = P.dma("sync", b1s, b1t, s_b)
        t_b2 = P.dma("sync", b2s, b2t, s_b)
        t_bias = t_b2
        s_xs = P.dma_sem("xs")
        s_w1 = [P.dma_sem("w1") for _ in range(NW1)]
        s_w2 = [P.dma_sem("w2") for _ in range(NW2)]
        s_y = [P.dma_sem("y") for _ in range(NYS)]

        w1_free = [None] * NW1
        w2_free = [None] * NW2
        xs_free = None
        tmp_free = [None] * NTMP
        ys_dma = [None] * NYS
        act_free = None
        nw1 = 0
        nw2 = 0
        ntmp = 0
        nys = 0
        psw = 0
        psy = 0
        ps_free = [None] * 8
        out_tokens = []

        def load_xs(e):
            nonlocal xs_free
            return P.dma("gpsimd", xs, xsT[e].rearrange("(k p) c -> p k c", p=128), s_xs, deps=[xs_free])

        def load_w1(e, u):
            nonlocal nw1
            slot = nw1 % NW1
            nw1 += 1
            src = w1[e].rearrange("(k p) n -> p k n", p=128)
            P.dma("gpsimd", w1u[slot][:, :, 0, :], src[:, :, 256 * u:256 * (u + 1)], s_w1[slot], deps=[w1_free[slot]])
            tok = P.dma("gpsimd", w1u[slot][:, :, 1, :], src[:, :, F + 256 * u:F + 256 * (u + 1)], s_w1[slot])
            return slot, tok

        def load_w2(e, u):
            nonlocal nw2
            slot = nw2 % NW2
            nw2 += 1
            src = w2[e].rearrange("(k p) n -> p k n", p=128)[:, :, 512 * u:512 * (u + 1)]
            tok = P.dma("gpsimd", w2u[slot], src, s_w2[slot], deps=[w2_free[slot]])
            return slot, tok

        plan = []
        for e in range(ne):
            for u in range(4):
                plan.append(("w1", e, u))
            for u in range(4 if dbg != 1 else 0):
                plan.append(("w2", e, u))
        issued = {}
        nissued = 0

        def ensure(idx):
            nonlocal nissued
            while nissued <= idx and nissued < len(plan):
                kind, e, u = plan[nissued]
                issued[nissued] = load_w1(e, u) if kind == "w1" else load_w2(e, u)
                nissued += 1

        t_xs = load_xs(0)
        pi = 0
        for e in range(ne):
            for u in range(4):
                ensure(pi + 2)
                slot, t_w = issued[pi]
                pi += 1
                last_mm = None
                for fcl in range(2):
                    fc = 2 * u + fcl
                    for (c0, cw) in blocks:
                        bA = 2 * (psw % 2)
                        bB = bA + 1
                        psw += 1
                        pA, pB = ps[bA], ps[bB]
                        for k in range(16):
                            last = P.op("tensor", lambda g, k=k, pA=pA, slot=slot, fcl=fcl, c0=c0, cw=cw: g.matmul(
                                pA[:, 0:cw], lhsT=w1u[slot][:, k, 0, 128 * fcl:128 * (fcl + 1)], rhs=xs[:, k, c0:c0 + cw],
                                start=(k == 0), stop=(k == 15)),
                                deps=[t_w, t_xs, ps_free[bA], act_free] if k == 0 else (), inc=(k == 15))
                        t_A = last
                        for k in range(16):
                            last = P.op("tensor", lambda g, k=k, pB=pB, slot=slot, fcl=fcl, c0=c0, cw=cw: g.matmul(
                                pB[:, 0:cw], lhsT=w1u[slot][:, k, 1, 128 * fcl:128 * (fcl + 1)], rhs=xs[:, k, c0:c0 + cw],
                                start=(k == 0), stop=(k == 15)),
                                deps=[ps_free[bB]] if k == 0 else (), inc=(k == 15))
                        t_B = last
                        last_mm = t_B
                        ti = ntmp % NTMP
                        ntmp += 1
                        t1 = P.op("vector", lambda g, pA=pA, ti=ti, e=e, fc=fc, cw=cw: g.tensor_scalar(
                            out=gl[ti][:, 0:cw], in0=pA[:, 0:cw], scalar1=b1s[:, e, fc:fc + 1], scalar2=7.0,
                            op0=ALU.add, op1=ALU.min), deps=[t_A, t_bias, tmp_free[ti]])
                        ps_free[bA] = t1
                        t2 = P.op("scalar", lambda g, ti=ti, cw=cw: g.activation(
                            out=sg[ti][:, 0:cw], in_=gl[ti][:, 0:cw], func=AF.Sigmoid, scale=1.702), deps=[t1, tmp_free[ti]])
                        t3 = P.op("scalar", lambda g, pB=pB, ti=ti, e=e, fc=fc, cw=cw: g.activation(
                            out=lb[ti][:, 0:cw], in_=pB[:, 0:cw], func=AF.Identity, bias=b1s[:, e, 8 + fc:9 + fc], scale=1.0),
                            deps=[t_B, t_bias])
                        ps_free[bB] = t3
                        t4 = P.op("vector", lambda g, ti=ti, cw=cw: g.tensor_scalar(
                            out=lb[ti][:, 0:cw], in0=lb[ti][:, 0:cw], scalar1=7.0, scalar2=-7.0, op0=ALU.min, op1=ALU.max),
                            deps=[t3])
                        t5 = P.op("vector", lambda g, ti=ti, cw=cw: g.tensor_tensor(
                            out=tt[ti][:, 0:cw], in0=gl[ti][:, 0:cw], in1=sg[ti][:, 0:cw], op=ALU.mult), deps=[t2, t4])
                        t6 = P.op("vector", lambda g, ti=ti, fc=fc, c0=c0, cw=cw: g.scalar_tensor_tensor(
                            out=act[:, fc, c0:c0 + cw], in0=lb[ti][:, 0:cw], scalar=1.0, in1=tt[ti][:, 0:cw],
                            op0=ALU.add, op1=ALU.mult), deps=[t5])
                        tmp_free[ti] = t6
                        t_act = t6
                w1_free[slot] = last_mm
            xs_free = last_mm
            if e + 1 < ne:
                t_xs = load_xs(e + 1)
            for u in range(4 if dbg != 1 else 0):
                ensure(pi + 2)
                slot, t_w = issued[pi]
                pi += 1
                last_mm = None
                for dcl in range(4):
                    dc = 4 * u + dcl
                    yi = nys % NYS
                    nys += 1
                    t_ev = None
                    for (c0, cw) in blocks:
                        bY = 4 + (psy % 2)
                        psy += 1
                        pY = ps[bY]
                        for k in range(8):
                            last = P.op("tensor", lambda g, k=k, pY=pY, slot=slot, dcl=dcl, c0=c0, cw=cw: g.matmul(
                                pY[:, 0:cw], lhsT=w2u[slot][:, k, 128 * dcl:128 * (dcl + 1)], rhs=act[:, k, c0:c0 + cw],
                                start=(k == 0), stop=(k == 7)),
                                deps=[t_w, t_act, ps_free[bY]] if k == 0 else (), inc=(k == 7))
                        last_mm = last
                        t_ev = P.op("scalar", lambda g, pY=pY, yi=yi, e=e, dc=dc, c0=c0, cw=cw: g.activation(
                            out=ys[yi][:, c0:c0 + cw], in_=pY[:, 0:cw], func=AF.Identity, bias=b2s[:, e, dc:dc + 1], scale=1.0),
                            deps=[last, t_bias, ys_dma[yi]])
                        ps_free[bY] = t_ev
                    if dbg != 2:
                        ys_dma[yi] = P.dma("gpsimd", yT[e, 128 * dc:128 * (dc + 1), :], ys[yi], s_y[yi], deps=[t_ev])
                    else:
                        P.wait("sync", [t_ev])
                w2_free[slot] = last_mm
            act_free = last_mm
        P.wait("gpsimd", [ys_dma[i] for i in range(NYS)] + [t_act])
        P.run()
    return nc


class LNUnit:
    def __init__(self, P, A, g_bc, b_bc, t_params, nset=2):
        self.P = P
        self.g_bc, self.b_bc, self.t_params = g_bc, b_bc, t_params
        self.nset = nset
        self.st = [A.alloc([24], F32) for _ in range(nset)]
        self.mv = [A.alloc([2], F32) for _ in range(nset)]
        self.rstd = [A.alloc([1], F32) for _ in range(nset)]
        self.nmr = [A.alloc([1], F32) for _ in range(nset)]
        self.eps = A.alloc([1], F32)
        self.t_eps = P.op("vector", lambda g: g.memset(self.eps, LN_EPS))
        self.free = [None] * nset
        self.n = 0

    def emit(self, z, o, deps, o_free=None, gb=None):
        for _ in self.emit_gen(z, o, deps, o_free, gb):
            pass
        return self.last

    def emit_gen(self, z, o, deps, o_free=None, gb=None):
        P = self.P
        i = self.n % self.nset
        self.n += 1
        st, mv, rstd, nmr = self.st[i], self.mv[i], self.rstd[i], self.nmr[i]
        g_bc, b_bc = gb if gb is not None else (self.g_bc, self.b_bc)
        if o_free is None:
            o_free = []
        elif isinstance(o_free, tuple):
            o_free = [o_free]
        t = None
        for c in range(4):
            t = P.op("vector", lambda g, c=c: g.bn_stats(out=st[:, 6 * c:6 * (c + 1)], in_=z[:, 512 * c:512 * (c + 1)]),
                     deps=list(deps) + [self.free[i]])
            yield
        t = P.op("vector", lambda g: g.bn_aggr(out=mv, in_=st), deps=[t])
        yield
        t = P.op("scalar", lambda g: g.activation(out=rstd, in_=mv[:, 1:2], func=AF.Sqrt, bias=self.eps[:, 0:1], scale=1.0),
                 deps=[t, self.t_eps])
        yield
        t = P.op("vector", lambda g: g.reciprocal(out=rstd, in_=rstd), deps=[t])
        yield
        t = P.op("vector", lambda g: g.tensor_scalar(out=nmr, in0=mv[:, 0:1], scalar1=rstd[:, 0:1], scalar2=-1.0,
                                                      op0=ALU.mult, op1=ALU.mult), deps=[t])
        yield
        t = P.op("scalar", lambda g: g.activation(out=o, in_=z, func=AF.Identity, scale=rstd[:, 0:1], bias=nmr[:, 0:1]),
                 deps=[t] + list(o_free))
        self.free[i] = t
        yield
        t = P.op("gpsimd", lambda g: g.tensor_tensor(out=o, in0=o, in1=g_bc, op=ALU.mult), deps=[t, self.t_params])
        yield
        t = P.op("gpsimd", lambda g: g.tensor_tensor(out=o, in0=o, in1=b_bc, op=ALU.add), deps=[t])
        self.last = t
        yield


def build_C(ntok=TOWN):
    nt = ntok // 128
    nc = bass.Bass("TRN2", target_bir_lowering=False)
    x1 = nc.dram_tensor("x1", [ntok, D], F32, kind="ExternalInput").ap()
    yk = nc.dram_tensor("yk", [ntok, TOPK, D], F32, kind="ExternalInput").ap()
    gk = nc.dram_tensor("gk", [ntok, TOPK], F32, kind="ExternalInput").ap()
    g_in = nc.dram_tensor("g_bc", [128, D], F32, kind="ExternalInput").ap()
    b_in = nc.dram_tensor("b_bc", [128, D], F32, kind="ExternalInput").ap()
    x2 = nc.dram_tensor("x2", [ntok, D], F32, kind="ExternalOutput").ap()
    with ExitStack() as es:
        big = es.enter_context(nc.sbuf_tensor("big", [128, SBUF_BYTES // 4], F32))
        P = Prog(nc, es)
        A = Bump(big, SBUF_BYTES)
        g_bc = A.alloc([D], F32)
        b_bc = A.alloc([D], F32)
        s_p = P.dma_sem("p")
        P.dma("sync", g_bc, g_in, s_p)
        t_params = P.dma("sync", b_bc, b_in, s_p)
        ln = LNUnit(P, A, g_bc, b_bc, t_params)
        NB = 2
        xin = [A.alloc([D], F32) for _ in range(NB)]
        yin = [A.alloc([TOPK, D], F32) for _ in range(NB)]
        gin = [A.alloc([TOPK], F32) for _ in range(NB)]
        ot = [A.alloc([D], F32) for _ in range(NB)]
        s_in = [P.dma_sem("in") for _ in range(NB)]
        s_out = [P.dma_sem("out") for _ in range(NB)]
        in_free = [None] * NB
        out_free = [None] * NB
        for j in range(nt):
            b = j % NB
            rows = slice(128 * j, 128 * (j + 1))
            P.dma("sync", xin[b], x1[rows, :], s_in[b], deps=[in_free[b]])
            P.dma("sync", gin[b], gk[rows, :], s_in[b])
            t_in = P.dma("sync", yin[b], yk[rows, :, :], s_in[b])
            t = P.op("scalar", lambda g, b=b: g.mul(xin[b], xin[b], ALPHA), deps=[t_in])
            for k in range(TOPK):
                t = P.op("vector", lambda g, b=b, k=k: g.scalar_tensor_tensor(
                    out=xin[b], in0=yin[b][:, k, :], scalar=gin[b][:, k:k + 1], in1=xin[b], op0=ALU.mult, op1=ALU.add),
                    deps=[t, t_in])
            t = ln.emit(xin[b], ot[b], [t], o_free=out_free[b])
            in_free[b] = t
            out_free[b] = P.dma("gpsimd", x2[rows, :], ot[b], s_out[b], deps=[t])
        P.wait("gpsimd", out_free)
        P.run()
    return nc


def pool_consts(core):
    tg = core * TOWN - HALO + np.arange(T)
    band = np.zeros((128, 4, NT, 3, 128), np.float32)
    invc = np.zeros((128, 4, T), np.float32)
    for g, w in enumerate(POOL_WINDOWS):
        lo = np.clip(tg - w // 2, 0, S)
        hi = np.clip(tg + w // 2, 0, S)
        cnt = np.maximum(hi - lo, 1)
        M = ((tg[:, None] >= lo[None, :]) & (tg[:, None] < hi[None, :])).astype(np.float32)
        M[np.arange(T), np.arange(T)] -= cnt
        invc[:, g, :] = (1.0 / cnt)[None, :]
        for j in range(NT):
            for r in range(3):
                jj = j + r - 1
                if 0 <= jj < NT:
                    band[:, g, j, r, :] = M[jj * 128:(jj + 1) * 128, j * 128:(j + 1) * 128]
    return band.reshape(128, 4, NT * 3, 128).astype(ml_dtypes.bfloat16), invc


def valid_mask(core):
    tg = core * TOWN - HALO + np.arange(T)
    return np.tile(((tg >= 0) & (tg < S)).astype(np.float32)[None, :], (128, 1))


def _emit_conv_phase(nc, P, A, ps, ps_free, ident, t_params, x, ins, stop=0):
    (w1_in, b1_in, wdw_in, bdw_in, lg_in, lb_in, w2_in, b2_in, mask_in) = ins
    TB = [(0, 384), (384, 384), (768, 384)]
    identb = A.alloc([128], BF16)
    ones = A.alloc([128], F32)
    cb1 = A.alloc([32], F32)
    wdw = A.alloc([16, CONV_W], F32)
    bdw = A.alloc([16], F32)
    clg = A.alloc([16], F32)
    clb = A.alloc([16], F32)
    eps = A.alloc([1], F32)
    s_c = P.dma_sem("cs")
    for dst, src in ((cb1, b1_in), (wdw, wdw_in), (bdw, bdw_in), (clg, lg_in)):
        P.dma("sync", dst, src, s_c)
    t_sm = P.dma("sync", clb, lb_in, s_c)
    P.op("vector", lambda g: g.memset(ones, 1.0))
    P.op("vector", lambda g: g.memset(eps, LN_EPS))
    t_idb = P.op("vector", lambda g: g.tensor_copy(out=identb, in_=ident), deps=[t_params])
    R1 = A.alloc([16, T], BF16)
    R2 = A.alloc([16, T], F32)
    m_R2 = A.mark() - 16 * T * 4
    m_R3 = A.mark()
    xT, uT, vT = R1, R1, R2
    xbf = A.alloc([NT, D], BF16)
    s_x = P.dma_sem("x")
    t_xbf = P.dma("gpsimd", xbf, x.rearrange("(j p) d -> p j d", p=128), s_x)
    n = 0
    t_a = None
    for j in range(NT):
        for h in range(2):
            bk = n % 2
            n += 1
            pb = ps[bk][:, :].bitcast(BF16)
            for i in range(8):
                k = 8 * h + i
                t_tr = P.op("tensor", lambda g, j=j, k=k, i=i, pb=pb: g.transpose(
                    out=pb[:, 128 * i:128 * (i + 1)], in_=xbf[:, j, 128 * k:128 * (k + 1)], identity=identb),
                    deps=[t_xbf, t_idb, ps_free[bk]] if i == 0 else (), inc=(i == 7))
            t_a = P.op("scalar" if n % 2 else "vector", lambda g, j=j, h=h, pb=pb: (g.copy if hasattr(g, "copy") else g.tensor_copy)(
                xT[:, 8 * h:8 * (h + 1), 128 * j:128 * (j + 1)], pb.rearrange("p (a c) -> p a c", a=8)), deps=[t_tr])
            ps_free[bk] = t_a
    t_a2 = P.op("vector", lambda g: g.memset(eps, LN_EPS), deps=[t_a, (P.eng["scalar"].sem, P.eng["scalar"].count)])
    t_a_done = t_a2
    if stop == 1:
        A.reset(m_R2)
        return t_a_done, None
    A.reset(m_R3)
    NW = 2
    w1u = [A.alloc([16, 2, 128], BF16) for _ in range(NW)]
    gpad = [A.alloc([T + 2 * CONV_PAD + 2], BF16) for _ in range(2)]
    Dg = [A.alloc([CONV_W, 128], BF16) for _ in range(2)]
    sgm = [A.alloc([384], F32) for _ in range(2)]
    mask = A.alloc([T], F32)
    s_m = P.dma_sem("m")
    t_mask = P.dma("sync", mask, mask_in, s_m, deps=[t_a_done])
    t_gz = None
    for gi in range(2):
        t_gz = P.op("vector", lambda g, gi=gi: g.memset(gpad[gi], 0.0), deps=[t_a_done])
    s_w1 = [P.dma_sem("cw1") for _ in range(NW)]
    w1_free = [t_a_done] * NW
    gpad_free = [t_gz] * 2
    Dg_free = [t_a_done] * 2
    sgm_free = [t_a_done] * 2
    w1v = w1_in.rearrange("(k p) n -> p k n", p=128)
    loads = {}

    def load_w1(i):
        sl = i % NW
        P.dma("gpsimd", w1u[sl][:, :, 0, :], w1v[:, :, 128 * i:128 * (i + 1)], s_w1[sl], deps=[w1_free[sl]])
        loads[i] = P.dma("gpsimd", w1u[sl][:, :, 1, :], w1v[:, :, D + 128 * i:D + 128 * (i + 1)], s_w1[sl])

    nW = 0
    nC = 0
    nsg = 0
    state = {}

    def emit_W(i):
        nonlocal nW, nsg
        sl = i % NW
        gi = i % 2
        di = i % 2
        t_d = None
        for k in range(CONV_W):
            t_d = P.op("vector", lambda g, di=di, k=k, i=i: g.tensor_scalar(
                out=Dg[di][:, k, :], in0=identb, scalar1=wdw[:, i, k:k + 1], scalar2=None, op0=ALU.mult),
                deps=[Dg_free[di], t_sm, t_idb] if k == 0 else ())
        t_gq = None
        t_mm = None
        for (c0, cw) in TB:
            bA = 2 + 2 * (nW % 2)
            bG = bA + 1
            nW += 1
            for k in range(16):
                t_A = P.op("tensor", lambda g, k=k, sl=sl, bA=bA, c0=c0, cw=cw: g.matmul(
                    ps[bA][:, 0:cw], lhsT=w1u[sl][:, k, 0, :], rhs=xT[:, k, c0:c0 + cw], start=(k == 0), stop=(k == 15)),
                    deps=[loads[i], t_a_done, ps_free[bA]] if k == 0 else (), inc=(k == 15))
            for k in range(16):
                t_G = P.op("tensor", lambda g, k=k, sl=sl, bG=bG, c0=c0, cw=cw: g.matmul(
                    ps[bG][:, 0:cw], lhsT=w1u[sl][:, k, 1, :], rhs=xT[:, k, c0:c0 + cw], start=(k == 0), stop=(k == 15)),
                    deps=[ps_free[bG]] if k == 0 else (), inc=(k == 15))
            t_mm = t_G
            si = nsg % 2
            nsg += 1
            t1 = P.op("scalar", lambda g, si=si, bG=bG, cw=cw, i=i: g.activation(
                out=sgm[si][:, 0:cw], in_=ps[bG][:, 0:cw], func=AF.Sigmoid, bias=cb1[:, 16 + i:17 + i], scale=1.0),
                deps=[t_G, t_sm, sgm_free[si]])
            ps_free[bG] = t1
            t2 = P.op("vector", lambda g, si=si, c0=c0, cw=cw: g.tensor_tensor(
                out=sgm[si][:, 0:cw], in0=sgm[si][:, 0:cw], in1=mask[:, c0:c0 + cw], op=ALU.mult), deps=[t1, t_mask])
            t_gq = P.op("vector", lambda g, si=si, gi=gi, bA=bA, c0=c0, cw=cw, i=i: g.scalar_tensor_tensor(
                out=gpad[gi][:, CONV_PAD + c0:CONV_PAD + c0 + cw], in0=ps[bA][:, 0:cw], scalar=cb1[:, i:i + 1],
                in1=sgm[si][:, 0:cw], op0=ALU.add, op1=ALU.mult), deps=[t2, t_A, gpad_free[gi]])
            ps_free[bA] = t_gq
            sgm_free[si] = t_gq
        w1_free[sl] = t_mm
        state[i] = (t_gq, t_d)

    t_v = None

    def emit_C(i):
        nonlocal nC, t_v
        gi = i % 2
        di = i % 2
        t_gq, t_d = state[i]
        t_mm = None
        for (c0, cw) in TB:
            bV = 6 + (nC % 2)
            nC += 1
            for k in range(CONV_W):
                t_mm = P.op("tensor", lambda g, k=k, di=di, gi=gi, bV=bV, c0=c0, cw=cw: g.matmul(
                    ps[bV][:, 0:cw], lhsT=Dg[di][:, k, :], rhs=gpad[gi][:, c0 + k:c0 + k + cw],
                    start=(k == 0), stop=(k == CONV_W - 1)),
                    deps=[t_gq, t_d, ps_free[bV]] if k == 0 else (), inc=(k == CONV_W - 1))
            t_v = P.op("scalar", lambda g, bV=bV, c0=c0, cw=cw, i=i: g.activation(
                out=vT[:, i, c0:c0 + cw], in_=ps[bV][:, 0:cw], func=AF.Identity, bias=bdw[:, i:i + 1], scale=1.0),
                deps=[t_mm, t_sm])
            ps_free[bV] = t_v
        gpad_free[gi] = t_mm
        Dg_free[di] = t_mm

    load_w1(0)
    for i in range(16):
        if i + 1 < 16:
            load_w1(i + 1)
        emit_W(i)
        if i >= 1:
            emit_C(i - 1)
    emit_C(15)
    t_bc_done = t_v
    if stop == 2:
        A.reset(m_R2)
        return t_bc_done, None
    A.reset(m_R3)
    mean = A.alloc([T], F32)
    rstd = A.alloc([T], F32)
    sq = [A.alloc([384], F32) for _ in range(2)]
    tmp = [A.alloc([T], F32) for _ in range(2)]
    sq_free = [t_bc_done] * 2
    t_st = None
    nsq = 0
    for (c0, cw) in TB:
        for i in range(16):
            t_s1 = P.op("tensor", lambda g, i=i, c0=c0, cw=cw: g.matmul(
                ps[0][:, 0:cw], lhsT=ones, rhs=vT[:, i, c0:c0 + cw], start=(i == 0), stop=(i == 15)),
                deps=[t_bc_done, ps_free[0]] if i == 0 else (), inc=(i == 15))
        for i in range(16):
            qi = nsq % 2
            nsq += 1
            t_q = P.op("scalar", lambda g, i=i, qi=qi, c0=c0, cw=cw: g.activation(
                out=sq[qi][:, 0:cw], in_=vT[:, i, c0:c0 + cw], func=AF.Square), deps=[t_bc_done, sq_free[qi]])
            t_s2 = P.op("tensor", lambda g, i=i, qi=qi, cw=cw: g.matmul(
                ps[1][:, 0:cw], lhsT=ones, rhs=sq[qi][:, 0:cw], start=(i == 0), stop=(i == 15)),
                deps=[t_q, ps_free[1]] if i == 0 else [t_q], inc=True)
            sq_free[qi] = t_s2
        t = P.op("vector", lambda g, c0=c0, cw=cw: g.tensor_scalar(
            out=mean[:, c0:c0 + cw], in0=ps[0][:, 0:cw], scalar1=1.0 / D, scalar2=None, op0=ALU.mult), deps=[t_s1, t_bc_done])
        ps_free[0] = t
        t = P.op("vector", lambda g, c0=c0, cw=cw: g.tensor_tensor(
            out=rstd[:, c0:c0 + cw], in0=mean[:, c0:c0 + cw], in1=mean[:, c0:c0 + cw], op=ALU.mult), deps=[t])
        t = P.op("vector", lambda g, c0=c0, cw=cw: g.scalar_tensor_tensor(
            out=rstd[:, c0:c0 + cw], in0=ps[1][:, 0:cw], scalar=1.0 / D, in1=rstd[:, c0:c0 + cw],
            op0=ALU.mult, op1=ALU.subtract), deps=[t, t_s2])
        ps_free[1] = t
        t = P.op("scalar", lambda g, c0=c0, cw=cw: g.activation(
            out=rstd[:, c0:c0 + cw], in_=rstd[:, c0:c0 + cw], func=AF.Sqrt, bias=eps[:, 0:1], scale=1.0), deps=[t])
        t_st = P.op("vector", lambda g, c0=c0, cw=cw: g.reciprocal(out=rstd[:, c0:c0 + cw], in_=rstd[:, c0:c0 + cw]), deps=[t])
    if stop == 3:
        A.reset(m_R2)
        return t_st, None
    tmp_free = [t_bc_done] * 2
    t_u = None
    for i in range(16):
        ti = i % 2
        t = P.op("vector", lambda g, i=i, ti=ti: g.tensor_tensor(out=tmp[ti], in0=vT[:, i, :], in1=mean, op=ALU.subtract),
                 deps=[t_st, tmp_free[ti]])
        t = P.op("gpsimd", lambda g, ti=ti: g.tensor_tensor(out=tmp[ti], in0=tmp[ti], in1=rstd, op=ALU.mult), deps=[t, t_st])
        t_u = P.op("scalar", lambda g, i=i, ti=ti: g.activation(
            out=uT[:, i, :], in_=tmp[ti], func=AF.Silu, scale=clg[:, i:i + 1], bias=clb[:, i:i + 1]), deps=[t, t_sm, t_bc_done])
        tmp_free[ti] = t_u
    t_d_done = P.op("vector", lambda g: g.memset(eps, LN_EPS), deps=[t_u])
    if stop == 4:
        A.reset(m_R2)
        return t_d_done, None
    A.reset(m_R3)
    NW2, NS = 2, 2
    w2u = [A.alloc([16, 512], BF16) for _ in range(NW2)]
    cb2 = A.alloc([D], F32)
    stg = [A.alloc([512], F32) for _ in range(NS)]
    mixd = nc.dram_tensor("mixd", [T, D], F32, kind="Internal").ap()
    s_w2 = [P.dma_sem("cw2") for _ in range(NW2)]
    s_st = [P.dma_sem("cst") for _ in range(NS)]
    s_b2 = P.dma_sem("cb2")
    w2_free = [t_d_done] * NW2
    stg_free = [None] * NS
    w2v = w2_in.rearrange("(k p) n -> p k n", p=128)

    def loadw2(nb):
        sl = nb % NW2
        return P.dma("gpsimd", w2u[sl], w2v[:, :, 512 * nb:512 * (nb + 1)], s_w2[sl], deps=[w2_free[sl]])

    tl = {0: loadw2(0), 1: loadw2(1)}
    t_b2 = P.dma("gpsimd", cb2, b2_in, s_b2, deps=[t_d_done])
    n = 0
    for nb in range(4):
        sl = nb % NW2
        t_mm = None
        for j in range(NT):
            bk = 2 + (n % 4)
            si = n % NS
            n += 1
            for k in range(16):
                t_mm = P.op("tensor", lambda g, k=k, sl=sl, bk=bk, j=j: g.matmul(
                    ps[bk][:, :], lhsT=uT[:, k, 128 * j:128 * (j + 1)], rhs=w2u[sl][:, k, :], start=(k == 0), stop=(k == 15)),
                    deps=[tl[nb], t_d_done, ps_free[bk]] if k == 0 else (), inc=(k == 15))
            t_e = P.op("vector", lambda g, nb=nb, bk=bk, si=si: g.tensor_tensor(
                out=stg[si], in0=ps[bk][:, :], in1=cb2[:, 512 * nb:512 * (nb + 1)], op=ALU.add),
                deps=[t_mm, t_b2, stg_free[si]])
            ps_free[bk] = t_e
            stg_free[si] = P.dma("gpsimd", mixd[128 * j:128 * (j + 1), 512 * nb:512 * (nb + 1)], stg[si], s_st[si], deps=[t_e])
        w2_free[sl] = t_mm
        if nb + 2 < 4:
            tl[nb + 2] = loadw2(nb + 2)
    t_mix_done = [stg_free[si] for si in range(NS)]
    t_d_done = P.op("vector", lambda g: g.memset(eps, LN_EPS), deps=[t_e])
    s_mx = [P.dma_sem("mx") for _ in range(2)]

    def tail(j, mixs_b, mix_free_b):
        return P.dma("sync", mixs_b, mixd[128 * j:128 * (j + 1), :], s_mx[j % 2], deps=[mix_free_b] + t_mix_done)

    A.reset(m_R2)
    A.nbytes = m_R3
    return t_d_done, tail


def build_A(kind, stop=0):
    nc = bass.Bass("TRN2", target_bir_lowering=False)
    inp = lambda name, shape, dt=F32: nc.dram_tensor(name, shape, dt, kind="ExternalInput").ap()
    x = inp("x", [T, D])
    ident_in = inp("ident", [128, 128])
    mg_in = inp("mg_bc", [128, D])
    mb_in = inp("mb_bc", [128, D])
    wr_in = inp("wr", [D, E])
    br_in = inp("br_bc", [128, E])
    if kind == "pool":
        band_in = inp("band", [128, 4, NT * 3, 128], BF16)
        invc_in = inp("invc", [128, 4, T])
        wp_in = inp("wp", [4, 512, 512])
        sc_in = inp("sc_bc", [128, D])
    else:
        w1_in = inp("cw1", [D, 2 * D])
        b1_in = inp("cb1t", [128, 32])
        wdw_in = inp("wdwt", [128, 16, CONV_W])
        bdw_in = inp("bdwt", [128, 16])
        lg_in = inp("clgt", [128, 16])
        lb_in = inp("clbt", [128, 16])
        w2_in = inp("cw2", [D, D])
        b2_in = inp("cb2_bc", [128, D])
        mask_in = inp("mask", [128, T])
    x1o = nc.dram_tensor("x1", [T, D], F32, kind="ExternalOutput").ap()
    Go = nc.dram_tensor("G", [T, E], F32, kind="ExternalOutput").ap()
    with ExitStack() as es:
        big = es.enter_context(nc.sbuf_tensor("big", [128, SBUF_BYTES // 4], F32))
        ps = [es.enter_context(nc.psum_tensor(f"ps{i}", [128, 512], F32)) for i in range(8)]
        P = Prog(nc, es)
        A = Bump(big, SBUF_BYTES)
        ps_free = [None] * 8
        ident = A.alloc([128], F32)
        mg_bc = A.alloc([D], F32)
        mb_bc = A.alloc([D], F32)
        wr_sb = A.alloc([16, E], F32)
        br_bc = A.alloc([E], F32)
        s_p = P.dma_sem("p")
        P.dma("sync", ident, ident_in, s_p)
        P.dma("sync", mg_bc, mg_in, s_p)
        P.dma("sync", mb_bc, mb_in, s_p)
        P.dma("sync", wr_sb, wr_in.rearrange("(k p) e -> p k e", p=128), s_p)
        t_params = P.dma("sync", br_bc, br_in, s_p)
        ln = LNUnit(P, A, mg_bc, mb_bc, t_params)
        s_x = P.dma_sem("x")
        if kind == "pool":
            pooledT = A.alloc([16, T], BF16)
            wpb = A.alloc([4, 4, 512], BF16)
            sc_bc = A.alloc([D], F32)
        m_phase = A.mark()
        if kind == "pool":
            xbf = A.alloc([NT, D], BF16)
            t_xbf = P.dma("gpsimd", xbf, x.rearrange("(j p) d -> p j d", p=128), s_x)

        if kind == "pool":
            s_w = P.dma_sem("w")
            for g in range(4):
                t_wp = P.dma("gpsimd", wpb[:, g, :, :], wp_in[g].rearrange("(k p) n -> p k n", p=128), s_w)
            s_sc = P.dma_sem("sc")
            t_sc = P.dma("sync", sc_bc, sc_in, s_sc)
            band = A.alloc([4, NT * 3, 128], BF16)
            invc = A.alloc([4, T], F32)
            s_bi = P.dma_sem("bi")
            P.dma("sync", band, band_in, s_bi)
            t_bi = P.dma("sync", invc, invc_in, s_bi)
            nbank = 0
            t_ev = None
            for cc in range(16):
                g = cc // 4
                for jb in range(0, NT, 4):
                    js = list(range(jb, min(jb + 4, NT)))
                    bk = nbank % 2
                    nbank += 1
                    first = True
                    for j in js:
                        rels = [r for r in range(3) if 0 <= j + r - 1 < NT]
                        for ri, r in enumerate(rels):
                            last = (j == js[-1] and ri == len(rels) - 1)
                            t_mm = P.op("tensor", lambda gg, cc=cc, g=g, j=j, r=r, bk=bk, jb=jb, ri=ri, n=len(rels): gg.matmul(
                                ps[bk][:, 128 * (j - jb):128 * (j - jb + 1)], lhsT=xbf[:, j + r - 1, 128 * cc:128 * (cc + 1)],
                                rhs=band[:, g, 3 * j + r, :], start=(ri == 0), stop=(ri == n - 1)),
                                deps=[t_xbf, t_bi, ps_free[bk]] if first else (), inc=last)
                            first = False
                    w = 128 * len(js)
                    t_ev = P.op("vector", lambda gg, cc=cc, g=g, bk=bk, jb=jb, w=w: gg.tensor_tensor(
                        out=pooledT[:, cc, 128 * jb:128 * jb + w], in0=ps[bk][:, 0:w], in1=invc[:, g, 128 * jb:128 * jb + w],
                        op=ALU.mult), deps=[t_mm, t_bi])
                    ps_free[bk] = t_ev
            t_phase1 = t_ev
            A.reset(m_phase)
        else:
            t_phase1, conv = _emit_conv_phase(nc, P, A, ps, ps_free, ident, t_params, x,
                                              (w1_in, b1_in, wdw_in, bdw_in, lg_in, lb_in, w2_in, b2_in, mask_in), stop)

        NB = 2
        xt = [A.alloc([D], F32) for _ in range(NB)]
        mixs = [A.alloc([D], F32) for _ in range(NB)]
        x1t = [A.alloc([D], F32) for _ in range(NB)]
        x1T = [A.alloc([16, 128], F32) for _ in range(NB)]
        lg = [A.alloc([E], F32) for _ in range(NB)]
        ex = [A.alloc([E], F32) for _ in range(NB)]
        top8 = [A.alloc([8], F32) for _ in range(NB)]
        sm = [A.alloc([4], F32) for _ in range(NB)]
        Gt = [A.alloc([E], F32) for _ in range(NB)]
        s_xt = [P.dma_sem("xt") for _ in range(NB)]
        s_o = [P.dma_sem("o") for _ in range(NB)]
        s_g = [P.dma_sem("g") for _ in range(NB)]
        xt_free = [t_phase1] * NB
        mix_free = [t_phase1] * NB
        x1t_free = [[t_phase1] for _ in range(NB)]
        x1T_free = [t_phase1] * NB
        sm_free = [t_phase1] * NB
        G_free = [None] * NB
        nps = [0]
        state = {}

        def front(j):
            b = j % NB
            rows = slice(128 * j, 128 * (j + 1))
            t_xt = P.dma("sync", xt[b], x[rows, :], s_xt[b], deps=[xt_free[b]])
            yield
            if kind == "pool":
                t_m = None
                for g in range(4):
                    bk = 2 + g
                    for kk in range(4):
                        t_mm = P.op("tensor", lambda gg, g=g, kk=kk, j=j, bk=bk: gg.matmul(
                            ps[bk][:, :], lhsT=pooledT[:, 4 * g + kk, 128 * j:128 * (j + 1)], rhs=wpb[:, g, kk, :],
                            start=(kk == 0), stop=(kk == 3)),
                            deps=[t_phase1, t_wp, ps_free[bk]] if kk == 0 else (), inc=(kk == 3))
                    t_m = P.op("vector", lambda gg, g=g, b=b, bk=bk: gg.tensor_tensor(
                        out=mixs[b][:, 512 * g:512 * (g + 1)], in0=ps[bk][:, :], in1=sc_bc[:, 512 * g:512 * (g + 1)], op=ALU.mult),
                        deps=[t_mm, t_sc, mix_free[b]])
                    ps_free[bk] = t_m
                    yield
            else:
                t_m = conv(j, mixs[b], mix_free[b])
                yield
            t_z = P.op("vector", lambda gg, b=b: gg.scalar_tensor_tensor(
                out=mixs[b], in0=xt[b], scalar=ALPHA, in1=mixs[b], op0=ALU.mult, op1=ALU.add), deps=[t_m, t_xt])
            xt_free[b] = t_z
            yield
            for _ in ln.emit_gen(mixs[b], x1t[b], [t_z], o_free=x1t_free[b]):
                yield
            t_x1 = ln.last
            mix_free[b] = t_x1
            t_st = P.dma("gpsimd", x1o[rows, :], x1t[b], s_o[b], deps=[t_x1])
            state[j] = (t_x1, t_st)
            yield

        def back(j):
            b = j % NB
            rows = slice(128 * j, 128 * (j + 1))
            t_x1, t_st = state[j]
            t_cp = None
            t_tr = None
            for q in range(4):
                bk = 6 + (nps[0] % 2)
                nps[0] += 1
                for i in range(4):
                    k = 4 * q + i
                    t_tr = P.op("tensor", lambda gg, b=b, k=k, i=i, bk=bk: gg.transpose(
                        out=ps[bk][:, 128 * i:128 * (i + 1)], in_=x1t[b][:, 128 * k:128 * (k + 1)], identity=ident),
                        deps=[t_x1, t_params, ps_free[bk]] if i == 0 else (), inc=(i == 3))
                t_cp = P.op("scalar", lambda gg, b=b, q=q, bk=bk: gg.copy(
                    x1T[b][:, 4 * q:4 * (q + 1), :], ps[bk][:, :].rearrange("p (a c) -> p a c", a=4)),
                    deps=[t_tr, x1T_free[b]])
                ps_free[bk] = t_cp
                yield
            x1t_free[b] = [t_st, t_tr]
            lbk = j % 2
            t_lg = None
            for k in range(16):
                t_lg = P.op("tensor", lambda gg, b=b, k=k, lbk=lbk: gg.matmul(
                    ps[lbk][:, 0:E], lhsT=x1T[b][:, k, :], rhs=wr_sb[:, k, :], start=(k == 0), stop=(k == 15)),
                    deps=[t_cp, ps_free[lbk], t_phase1] if k == 0 else (), inc=(k == 15))
            x1T_free[b] = t_lg
            yield
            t = P.op("vector", lambda gg, b=b, lbk=lbk: gg.tensor_tensor(out=lg[b], in0=ps[lbk][:, 0:E], in1=br_bc, op=ALU.add),
                     deps=[t_lg, t_params, sm_free[b], G_free[b]])
            ps_free[lbk] = t
            yield
            t = P.op("vector", lambda gg, b=b: gg.max(out=top8[b], in_=lg[b]), deps=[t])
            yield
            t = P.op("vector", lambda gg, b=b: gg.tensor_scalar(out=sm[b][:, 0:1], in0=top8[b][:, 0:1], scalar1=-1.0, scalar2=None,
                                                                 op0=ALU.mult), deps=[t])
            yield
            t = P.op("scalar", lambda gg, b=b: gg.activation(out=ex[b], in_=lg[b], func=AF.Exp, bias=sm[b][:, 0:1], scale=1.0),
                     deps=[t])
            yield
            t = P.op("vector", lambda gg, b=b: gg.scalar_tensor_tensor(
                out=ex[b], in0=lg[b], scalar=top8[b][:, 3:4], in1=ex[b], op0=ALU.is_ge, op1=ALU.mult), deps=[t])
            yield
            t = P.op("vector", lambda gg, b=b: gg.tensor_reduce(out=sm[b][:, 1:2], in_=ex[b], axis=mybir.AxisListType.X, op=ALU.add),
                     deps=[t])
            yield
            t = P.op("vector", lambda gg, b=b: gg.reciprocal(out=sm[b][:, 2:3], in_=sm[b][:, 1:2]), deps=[t])
            yield
            t = P.op("vector", lambda gg, b=b: gg.tensor_scalar(out=Gt[b], in0=ex[b], scalar1=sm[b][:, 2:3], scalar2=None,
                                                                 op0=ALU.mult), deps=[t])
            sm_free[b] = t
            G_free[b] = P.dma("gpsimd", Go[rows, :], Gt[b], s_g[b], deps=[t])
            yield

        ntail = NT if not stop else 0
        for j in range(ntail + 1):
            gens = []
            if j < ntail:
                gens.append(front(j))
            if 1 <= j <= ntail:
                gens.append(back(j - 1))
            while gens:
                for gnr in list(gens):
                    try:
                        next(gnr)
                    except StopIteration:
                        gens.remove(gnr)
        P.wait("gpsimd", [G_free[i] for i in range(NB)] + [(s_o[i][0], s_o[i][1]) for i in range(NB) if s_o[i][1]] + [t_phase1])
        P.run()
    return nc


def _bc(v):
    return np.ascontiguousarray(np.broadcast_to(np.asarray(v, np.float32)[None, :], (128, v.shape[0])))


def _slice_halo(xfull, core):
    out = np.zeros((T, D), np.float32)
    lo = core * TOWN - HALO
    a, b = max(lo, 0), min(lo + T, S)
    out[a - lo:b - lo] = xfull[a:b]
    return out


def _chunk_t(v, n):
    return np.ascontiguousarray(np.asarray(v, np.float32).reshape(n, 128).T)


_IDENT = np.eye(128, dtype=np.float32)
_POOLC = {}


def inputs_A_pool(core, xfull, pool_w, pool_scale, mg, mb, wr, br):
    if core not in _POOLC:
        _POOLC[core] = pool_consts(core)
    band, invc = _POOLC[core]
    return {"x": _slice_halo(xfull, core), "ident": _IDENT, "mg_bc": _bc(mg), "mb_bc": _bc(mb), "wr": np.ascontiguousarray(wr),
            "br_bc": _bc(br), "band": band, "invc": invc, "wp": np.ascontiguousarray(pool_w), "sc_bc": _bc(pool_scale)}


def inputs_A_conv(core, xfull, w1, b1, wdw, bdw, lng, lnb, w2, b2, mg, mb, wr, br):
    wdwt = np.ascontiguousarray(np.asarray(wdw, np.float32).T.reshape(16, 128, CONV_W).transpose(1, 0, 2))
    return {"x": _slice_halo(xfull, core), "ident": _IDENT, "mg_bc": _bc(mg), "mb_bc": _bc(mb), "wr": np.ascontiguousarray(wr),
            "br_bc": _bc(br), "cw1": np.ascontiguousarray(w1), "cb1t": _chunk_t(b1, 32), "wdwt": wdwt,
            "bdwt": _chunk_t(bdw, 16), "clgt": _chunk_t(lng, 16), "clbt": _chunk_t(lnb, 16),
            "cw2": np.ascontiguousarray(w2), "cb2_bc": _bc(b2), "mask": valid_mask(core)}


_PROGS = {}


def _prog(key, builder):
    if key not in _PROGS:
        _PROGS[key] = builder()
    return _PROGS[key]


def _run(nc, in_maps):
    res = run_bass_kernel_spmd(nc, in_maps, core_ids=list(range(NCORES)))
    return res.results


def _lay_bias(b):
    ne = b.shape[0]
    return np.ascontiguousarray(np.asarray(b, np.float32).reshape(ne, 16, 128).transpose(2, 0, 1))


def kernel(x, pool_w, pool_scale, conv_w1, conv_b1, conv_wdw, conv_bdw, conv_ln_g, conv_ln_b, conv_w2, conv_b2,
           mix_ln_g, mix_ln_b, router_w, router_b, moe_w1, moe_b1, moe_w2, moe_b2, ffn_ln_g, ffn_ln_b):
    f32 = lambda a: np.asarray(a, dtype=np.float32)
    xcur = np.ascontiguousarray(f32(x)[0])
    cen = slice(HALO, HALO + TOWN)
    for i in range(DEPTH):
        j = i // 2
        if i % 2 == 0:
            ncA = _prog("A_pool", lambda: build_A("pool"))
            maps = [inputs_A_pool(c, xcur, f32(pool_w[j]), f32(pool_scale[j]), f32(mix_ln_g[i]), f32(mix_ln_b[i]),
                                  f32(router_w[i]), f32(router_b[i])) for c in range(NCORES)]
        else:
            ncA = _prog("A_conv", lambda: build_A("conv"))
            maps = [inputs_A_conv(c, xcur, f32(conv_w1[j]), f32(conv_b1[j]), f32(conv_wdw[j]), f32(conv_bdw[j]),
                                  f32(conv_ln_g[j]), f32(conv_ln_b[j]), f32(conv_w2[j]), f32(conv_b2[j]),
                                  f32(mix_ln_g[i]), f32(mix_ln_b[i]), f32(router_w[i]), f32(router_b[i]))
                    for c in range(NCORES)]
        res = _run(ncA, maps)
        del maps
        x1 = np.concatenate([r["x1"][cen] for r in res], axis=0)
        G = np.concatenate([r["G"][cen] for r in res], axis=0)
        del res
        sel = G > 0
        toks = [np.nonzero(sel[:, e])[0] for e in range(E)]
        cmax = max(int(t.shape[0]) for t in toks)
        C = max(128, (cmax + 127) // 128 * 128)
        kpos = np.cumsum(sel, axis=1) - 1
        maps = []
        for c in range(NCORES):
            xsT = np.zeros((NE, D, C), np.float32)
            for m in range(NE):
                t = toks[c + NCORES * m]
                xsT[m, :, :t.shape[0]] = x1[t].T
            maps.append({"xsT": xsT, "w1": f32(moe_w1[i][c::NCORES]), "b1t": _lay_bias(f32(moe_b1[i][c::NCORES])),
                         "w2": f32(moe_w2[i][c::NCORES]), "b2t": _lay_bias(f32(moe_b2[i][c::NCORES]))})
        ncB = _prog(("B", C), lambda: build_B(C))
        res = _run(ncB, maps)
        del maps
        yk = np.zeros((S, TOPK, D), np.float32)
        gk = np.zeros((S, TOPK), np.float32)
        for c in range(NCORES):
            for m in range(NE):
                e = c + NCORES * m
                t = toks[e]
                kp = np.minimum(kpos[t, e], TOPK - 1)
                yk[t, kp, :] = res[c]["yT"][m][:, :t.shape[0]].T
                gk[t, kp] = G[t, e]
        del res
        ncC = _prog("C", lambda: build_C(TOWN))
        gb, bb = _bc(f32(ffn_ln_g[i])), _bc(f32(ffn_ln_b[i]))
        maps = [{"x1": x1[c * TOWN:(c + 1) * TOWN], "yk": yk[c * TOWN:(c + 1) * TOWN], "gk": gk[c * TOWN:(c + 1) * TOWN],
                 "g_bc": gb, "b_bc": bb} for c in range(NCORES)]
        res = _run(ncC, maps)
        del maps, yk
        xcur = np.concatenate([r["x2"] for r in res], axis=0)
        del res
    return np.ascontiguousarray(xcur[None].astype(np.float32))
```

```python
import numpy as np
import ml_dtypes
from contextlib import ExitStack
import concourse.bass as bass
import concourse.mybir as mybir
from concourse.bass_utils import run_bass_kernel_spmd

F32 = mybir.dt.float32
BF16 = mybir.dt.bfloat16
ALU = mybir.AluOpType
AF = mybir.ActivationFunctionType

D = 2048
S = 8192
NCORES = 8
TOWN = S // NCORES
HALO = 64
T = TOWN + 2 * HALO
NT = T // 128
E = 32
TOPK = 4
F = 1024
NE = E // NCORES
DEPTH = 4
ALPHA = (2.0 * DEPTH) ** 0.25
LN_EPS = 1e-5
CONV_W = 31
CONV_PAD = 15
POOL_WINDOWS = (2, 4, 8, 16)


class _Eng:
    def __init__(self, name, sem):
        self.name = name
        self.sem = sem
        self.count = 0
        self.q = []
        self.waited = {}


class Prog:
    ENGS = ("sync", "scalar", "vector", "gpsimd", "tensor")

    def __init__(self, nc, es):
        self.nc = nc
        self.es = es
        self.eng = {n: _Eng(n, es.enter_context(nc.semaphore("po_" + n))) for n in self.ENGS}
        self.nsem = 0

    def dma_sem(self, name):
        self.nsem += 1
        return [self.es.enter_context(self.nc.semaphore(f"d_{name}_{self.nsem}")), 0]

    def _waits(self, e, deps):
        for d in deps:
            if d is None:
                continue
            s, v = d
            key = id(s)
            if e.waited.get(key, 0) < v:
                e.waited[key] = v
                e.q.append(lambda g, s=s, v=v: g.wait_ge(s, v))

    def op(self, eng, fn, deps=(), inc=True):
        e = self.eng[eng]
        self._waits(e, deps)
        if inc:
            e.count += 1
            sem = e.sem
            e.q.append(lambda g, fn=fn, sem=sem: fn(g).then_inc(sem, 1))
            return (e.sem, e.count)
        e.q.append(lambda g, fn=fn: fn(g))
        return None

    def dma(self, eng, out, in_, slot, deps=()):
        e = self.eng[eng]
        self._waits(e, deps)
        slot[1] += 16
        sem = slot[0]
        e.q.append(lambda g, out=out, in_=in_, sem=sem: g.dma_start(out=out, in_=in_).then_inc(sem, 16))
        return (sem, slot[1])

    def wait(self, eng, deps):
        self._waits(self.eng[eng], deps)

    def run(self):
        nc = self.nc
        with nc.Block() as block:
            for name in self.ENGS:
                q = self.eng[name].q

                def body(g, q=q):
                    for fn in q:
                        fn(g)
                getattr(block, name)(body)


class Bump:
    def __init__(self, big, nbytes):
        self.big = big
        self.nbytes = nbytes
        self.off = 0

    def alloc(self, shape_free, dtype):
        esz = 4 if dtype == F32 else 2
        n = int(np.prod(shape_free))
        nb = (n * esz + 31) // 32 * 32
        assert self.off + nb <= self.nbytes, f"SBUF overflow {self.off + nb} > {self.nbytes}"
        v = self.big[:, self.off // 4:(self.off + nb) // 4]
        self.off += nb
        if dtype != F32:
            v = v.bitcast(dtype)
        v = v[:, 0:n]
        if len(shape_free) == 2:
            v = v.rearrange("p (a b) -> p a b", a=shape_free[0])
        elif len(shape_free) == 3:
            v = v.rearrange("p (a b c) -> p a b c", a=shape_free[0], b=shape_free[1])
        return v

    def mark(self):
        return self.off

    def reset(self, m):
        self.off = m


SBUF_BYTES = 176 * 1024


def _blocks(C):
    nt = C // 128
    nb = (C + 511) // 512
    if nt % nb == 0:
        w = C // nb
        return [(i * w, w) for i in range(nb)]
    out, c0 = [], 0
    while c0 < C:
        w = min(512, C - c0)
        out.append((c0, w))
        c0 += w
    return out


def build_B(Cs, ne=None, dbg=0):
    if isinstance(Cs, int):
        Cs = (Cs,) * (ne if ne is not None else NE)
    Cs = tuple(int(c) for c in Cs)
    ne = len(Cs)
    C = max(Cs)
    nc = bass.Bass("TRN2", target_bir_lowering=False)
    xsTs = [nc.dram_tensor(f"xsT{m}", [D, Cs[m]], F32, kind="ExternalInput").ap() for m in range(ne)]
    w1 = nc.dram_tensor("w1", [ne, D, 2 * F], F32, kind="ExternalInput").ap()
    b1t = nc.dram_tensor("b1t", [128, ne, 16], F32, kind="ExternalInput").ap()
    w2 = nc.dram_tensor("w2", [ne, F, D], F32, kind="ExternalInput").ap()
    b2t = nc.dram_tensor("b2t", [128, ne, 16], F32, kind="ExternalInput").ap()
    yTs = [nc.dram_tensor(f"yT{m}", [D, Cs[m]], F32, kind="ExternalOutput").ap() for m in range(ne)]
    eblocks = [_blocks(c) for c in Cs]
    W = max(w for bl in eblocks for _, w in bl)
    with ExitStack() as es:
        big = es.enter_context(nc.sbuf_tensor("big", [128, SBUF_BYTES // 4], F32))
        ps = [es.enter_context(nc.psum_tensor(f"ps{i}", [128, 512], F32)) for i in range(8)]
        P = Prog(nc, es)
        A = Bump(big, SBUF_BYTES)
        xs = A.alloc([16, C], BF16)
        act = A.alloc([8, C], BF16)
        NW1 = 3
        w1u = [A.alloc([16, 2, 256], BF16) for _ in range(NW1)]
        NW2 = 3
        w2u = [A.alloc([8, 512], BF16) for _ in range(NW2)]
        b1s = A.alloc([ne, 16], F32)
        b2s = A.alloc([ne, 16], F32)
        NTMP = 2
        gl = [A.alloc([W], F32) for _ in range(NTMP)]
        sg = [A.alloc([W], F32) for _ in range(NTMP)]
        lb = [A.alloc([W], F32) for _ in range(NTMP)]
        tt = [A.alloc([W], F32) for _ in range(NTMP)]
        NYS = 2
        ys = [A.alloc([C], F32) for _ in range(NYS)]

        s_b = P.dma_sem("b")
        t_b1 = P.dma("sync", b1s, b1t, s_b)
        t_b2 = P.dma("sync", b2s, b2t, s_b)
        t_bias = t_b2
        s_xs = P.dma_sem("xs")
        s_w1 = [P.dma_sem("w1") for _ in range(NW1)]
        s_w2 = [P.dma_sem("w2") for _ in range(NW2)]
        s_y = [P.dma_sem("y") for _ in range(NYS)]

        w1_free = [None] * NW1
        w2_free = [None] * NW2
        xs_free = None
        tmp_free = [None] * NTMP
        ys_dma = [None] * NYS
        act_free = None
        nw1 = 0
        nw2 = 0
        ntmp = 0
        nys = 0
        psw = 0
        psy = 0
        ps_free = [None] * 8
        out_tokens = []

        def load_xs(e):
            nonlocal xs_free
            return P.dma("gpsimd", xs[:, :, 0:Cs[e]], xsTs[e].rearrange("(k p) c -> p k c", p=128), s_xs, deps=[xs_free])

        def load_w1(e, u):
            nonlocal nw1
            slot = nw1 % NW1
            nw1 += 1
            src = w1[e].rearrange("(k p) n -> p k n", p=128)
            P.dma("gpsimd", w1u[slot][:, :, 0, :], src[:, :, 256 * u:256 * (u + 1)], s_w1[slot], deps=[w1_free[slot]])
            tok = P.dma("gpsimd", w1u[slot][:, :, 1, :], src[:, :, F + 256 * u:F + 256 * (u + 1)], s_w1[slot])
            return slot, tok

        def load_w2(e, u):
            nonlocal nw2
            slot = nw2 % NW2
            nw2 += 1
            src = w2[e].rearrange("(k p) n -> p k n", p=128)[:, :, 512 * u:512 * (u + 1)]
            tok = P.dma("gpsimd", w2u[slot], src, s_w2[slot], deps=[w2_free[slot]])
            return slot, tok

        plan = []
        for e in range(ne):
            for u in range(4):
                plan.append(("w1", e, u))
            for u in range(4 if dbg != 1 else 0):
                plan.append(("w2", e, u))
        issued = {}
        nissued = 0

        def ensure(idx):
            nonlocal nissued
            while nissued <= idx and nissued < len(plan):
                kind, e, u = plan[nissued]
                issued[nissued] = load_w1(e, u) if kind == "w1" else load_w2(e, u)
                nissued += 1

        t_xs = load_xs(0)
        pi = 0
        for e in range(ne):
            blocks = eblocks[e]
            for u in range(4):
                ensure(pi + 2)
                slot, t_w = issued[pi]
                pi += 1
                last_mm = None
                for fcl in range(2):
                    fc = 2 * u + fcl
                    for (c0, cw) in blocks:
                        bA = 2 * (psw % 2)
                        bB = bA + 1
                        psw += 1
                        pA, pB = ps[bA], ps[bB]
                        for k in range(16):
                            last = P.op("tensor", lambda g, k=k, pA=pA, slot=slot, fcl=fcl, c0=c0, cw=cw: g.matmul(
                                pA[:, 0:cw], lhsT=w1u[slot][:, k, 0, 128 * fcl:128 * (fcl + 1)], rhs=xs[:, k, c0:c0 + cw],
                                start=(k == 0), stop=(k == 15)),
                                deps=[t_w, t_xs, ps_free[bA], act_free] if k == 0 else (), inc=(k == 15))
                        t_A = last
                        for k in range(16):
                            last = P.op("tensor", lambda g, k=k, pB=pB, slot=slot, fcl=fcl, c0=c0, cw=cw: g.matmul(
                                pB[:, 0:cw], lhsT=w1u[slot][:, k, 1, 128 * fcl:128 * (fcl + 1)], rhs=xs[:, k, c0:c0 + cw],
                                start=(k == 0), stop=(k == 15)),
                                deps=[ps_free[bB]] if k == 0 else (), inc=(k == 15))
                        t_B = last
                        last_mm = t_B
                        ti = ntmp % NTMP
                        ntmp += 1
                        t1 = P.op("vector", lambda g, pA=pA, ti=ti, e=e, fc=fc, cw=cw: g.tensor_scalar(
                            out=gl[ti][:, 0:cw], in0=pA[:, 0:cw], scalar1=b1s[:, e, fc:fc + 1], scalar2=7.0,
                            op0=ALU.add, op1=ALU.min), deps=[t_A, t_bias, tmp_free[ti]])
                        ps_free[bA] = t1
                        t2 = P.op("scalar", lambda g, ti=ti, cw=cw: g.activation(
                            out=sg[ti][:, 0:cw], in_=gl[ti][:, 0:cw], func=AF.Sigmoid, scale=1.702), deps=[t1, tmp_free[ti]])
                        t3 = P.op("scalar", lambda g, pB=pB, ti=ti, e=e, fc=fc, cw=cw: g.activation(
                            out=lb[ti][:, 0:cw], in_=pB[:, 0:cw], func=AF.Identity, bias=b1s[:, e, 8 + fc:9 + fc], scale=1.0),
                            deps=[t_B, t_bias])
                        ps_free[bB] = t3
                        t4 = P.op("vector", lambda g, ti=ti, cw=cw: g.tensor_scalar(
                            out=lb[ti][:, 0:cw], in0=lb[ti][:, 0:cw], scalar1=7.0, scalar2=-7.0, op0=ALU.min, op1=ALU.max),
                            deps=[t3])
                        t5 = P.op("vector", lambda g, ti=ti, cw=cw: g.tensor_tensor(
                            out=tt[ti][:, 0:cw], in0=gl[ti][:, 0:cw], in1=sg[ti][:, 0:cw], op=ALU.mult), deps=[t2, t4])
                        t6 = P.op("vector", lambda g, ti=ti, fc=fc, c0=c0, cw=cw: g.scalar_tensor_tensor(
                            out=act[:, fc, c0:c0 + cw], in0=lb[ti][:, 0:cw], scalar=1.0, in1=tt[ti][:, 0:cw],
                            op0=ALU.add, op1=ALU.mult), deps=[t5])
                        tmp_free[ti] = t6
                        t_act = t6
                w1_free[slot] = last_mm
            xs_free = last_mm
            if e + 1 < ne:
                t_xs = load_xs(e + 1)
            for u in range(4 if dbg != 1 else 0):
                ensure(pi + 2)
                slot, t_w = issued[pi]
                pi += 1
                last_mm = None
                for dcl in range(4):
                    dc = 4 * u + dcl
                    yi = nys % NYS
                    nys += 1
                    t_ev = None
                    for (c0, cw) in blocks:
                        bY = 4 + (psy % 2)
                        psy += 1
                        pY = ps[bY]
                        for k in range(8):
                            last = P.op("tensor", lambda g, k=k, pY=pY, slot=slot, dcl=dcl, c0=c0, cw=cw: g.matmul(
                                pY[:, 0:cw], lhsT=w2u[slot][:, k, 128 * dcl:128 * (dcl + 1)], rhs=act[:, k, c0:c0 + cw],
                                start=(k == 0), stop=(k == 7)),
                                deps=[t_w, t_act, ps_free[bY]] if k == 0 else (), inc=(k == 7))
                        last_mm = last
                        t_ev = P.op("scalar", lambda g, pY=pY, yi=yi, e=e, dc=dc, c0=c0, cw=cw: g.activation(
                            out=ys[yi][:, c0:c0 + cw], in_=pY[:, 0:cw], func=AF.Identity, bias=b2s[:, e, dc:dc + 1], scale=1.0),
                            deps=[last, t_bias, ys_dma[yi]])
                        ps_free[bY] = t_ev
                    if dbg != 2:
                        ys_dma[yi] = P.dma("gpsimd", yTs[e][128 * dc:128 * (dc + 1), :], ys[yi][:, 0:Cs[e]], s_y[yi], deps=[t_ev])
                    else:
                        P.wait("sync", [t_ev])
                w2_free[slot] = last_mm
            act_free = last_mm
        P.wait("gpsimd", [ys_dma[i] for i in range(NYS)] + [t_act])
        P.run()
    return nc


class LNUnit:
    def __init__(self, P, A, g_bc, b_bc, t_params, nset=2):
        self.P = P
        self.g_bc, self.b_bc, self.t_params = g_bc, b_bc, t_params
        self.nset = nset
        self.st = [A.alloc([24], F32) for _ in range(nset)]
        self.mv = [A.alloc([2], F32) for _ in range(nset)]
        self.rstd = [A.alloc([1], F32) for _ in range(nset)]
        self.nmr = [A.alloc([1], F32) for _ in range(nset)]
        self.eps = A.alloc([1], F32)
        self.t_eps = P.op("vector", lambda g: g.memset(self.eps, LN_EPS))
        self.free = [None] * nset
        self.n = 0

    def emit(self, z, o, deps, o_free=None, gb=None):
        for _ in self.emit_gen(z, o, deps, o_free, gb):
            pass
        return self.last

    def emit_gen(self, z, o, deps, o_free=None, gb=None):
        P = self.P
        i = self.n % self.nset
        self.n += 1
        st, mv, rstd, nmr = self.st[i], self.mv[i], self.rstd[i], self.nmr[i]
        g_bc, b_bc = gb if gb is not None else (self.g_bc, self.b_bc)
        if o_free is None:
            o_free = []
        elif isinstance(o_free, tuple):
            o_free = [o_free]
        t = None
        for c in range(4):
            t = P.op("vector", lambda g, c=c: g.bn_stats(out=st[:, 6 * c:6 * (c + 1)], in_=z[:, 512 * c:512 * (c + 1)]),
                     deps=list(deps) + [self.free[i]])
            yield
        t = P.op("vector", lambda g: g.bn_aggr(out=mv, in_=st), deps=[t])
        yield
        t = P.op("scalar", lambda g: g.activation(out=rstd, in_=mv[:, 1:2], func=AF.Sqrt, bias=self.eps[:, 0:1], scale=1.0),
                 deps=[t, self.t_eps])
        yield
        t = P.op("vector", lambda g: g.reciprocal(out=rstd, in_=rstd), deps=[t])
        yield
        t = P.op("vector", lambda g: g.tensor_scalar(out=nmr, in0=mv[:, 0:1], scalar1=rstd[:, 0:1], scalar2=-1.0,
                                                      op0=ALU.mult, op1=ALU.mult), deps=[t])
        yield
        t = P.op("scalar", lambda g: g.activation(out=o, in_=z, func=AF.Identity, scale=rstd[:, 0:1], bias=nmr[:, 0:1]),
                 deps=[t] + list(o_free))
        self.free[i] = t
        yield
        t = P.op("gpsimd", lambda g: g.tensor_tensor(out=o, in0=o, in1=g_bc, op=ALU.mult), deps=[t, self.t_params])
        yield
        t = P.op("gpsimd", lambda g: g.tensor_tensor(out=o, in0=o, in1=b_bc, op=ALU.add), deps=[t])
        self.last = t
        yield


def build_C(ntok=TOWN):
    nt = ntok // 128
    nc = bass.Bass("TRN2", target_bir_lowering=False)
    x1 = nc.dram_tensor("x1", [ntok, D], F32, kind="ExternalInput").ap()
    yk = nc.dram_tensor("yk", [ntok, TOPK, D], F32, kind="ExternalInput").ap()
    gk = nc.dram_tensor("gk", [ntok, TOPK], F32, kind="ExternalInput").ap()
    g_in = nc.dram_tensor("g_bc", [128, D], F32, kind="ExternalInput").ap()
    b_in = nc.dram_tensor("b_bc", [128, D], F32, kind="ExternalInput").ap()
    x2 = nc.dram_tensor("x2", [ntok, D], F32, kind="ExternalOutput").ap()
    with ExitStack() as es:
        big = es.enter_context(nc.sbuf_tensor("big", [128, SBUF_BYTES // 4], F32))
        P = Prog(nc, es)
        A = Bump(big, SBUF_BYTES)
        g_bc = A.alloc([D], F32)
        b_bc = A.alloc([D], F32)
        s_p = P.dma_sem("p")
        P.dma("sync", g_bc, g_in, s_p)
        t_params = P.dma("sync", b_bc, b_in, s_p)
        ln = LNUnit(P, A, g_bc, b_bc, t_params)
        NB = 2
        xin = [A.alloc([D], F32) for _ in range(NB)]
        yin = [A.alloc([TOPK, D], F32) for _ in range(NB)]
        gin = [A.alloc([TOPK], F32) for _ in range(NB)]
        ot = [A.alloc([D], F32) for _ in range(NB)]
        s_in = [P.dma_sem("in") for _ in range(NB)]
        s_out = [P.dma_sem("out") for _ in range(NB)]
        in_free = [None] * NB
        out_free = [None] * NB
        for j in range(nt):
            b = j % NB
            rows = slice(128 * j, 128 * (j + 1))
            P.dma("sync", xin[b], x1[rows, :], s_in[b], deps=[in_free[b]])
            P.dma("sync", gin[b], gk[rows, :], s_in[b])
            t_in = P.dma("sync", yin[b], yk[rows, :, :], s_in[b])
            t = P.op("scalar", lambda g, b=b: g.mul(xin[b], xin[b], ALPHA), deps=[t_in])
            for k in range(TOPK):
                t = P.op("vector", lambda g, b=b, k=k: g.scalar_tensor_tensor(
                    out=xin[b], in0=yin[b][:, k, :], scalar=gin[b][:, k:k + 1], in1=xin[b], op0=ALU.mult, op1=ALU.add),
                    deps=[t, t_in])
            t = ln.emit(xin[b], ot[b], [t], o_free=out_free[b])
            in_free[b] = t
            out_free[b] = P.dma("gpsimd", x2[rows, :], ot[b], s_out[b], deps=[t])
        P.wait("gpsimd", out_free)
        P.run()
    return nc


def pool_consts(core):
    tg = core * TOWN - HALO + np.arange(T)
    band = np.zeros((128, 4, NT, 3, 128), np.float32)
    invc = np.zeros((128, 4, T), np.float32)
    for g, w in enumerate(POOL_WINDOWS):
        lo = np.clip(tg - w // 2, 0, S)
        hi = np.clip(tg + w // 2, 0, S)
        cnt = np.maximum(hi - lo, 1)
        M = ((tg[:, None] >= lo[None, :]) & (tg[:, None] < hi[None, :])).astype(np.float32)
        M[np.arange(T), np.arange(T)] -= cnt
        invc[:, g, :] = (1.0 / cnt)[None, :]
        for j in range(NT):
            for r in range(3):
                jj = j + r - 1
                if 0 <= jj < NT:
                    band[:, g, j, r, :] = M[jj * 128:(jj + 1) * 128, j * 128:(j + 1) * 128]
    return band.reshape(128, 4, NT * 3, 128).astype(ml_dtypes.bfloat16), invc


def valid_mask(core):
    tg = core * TOWN - HALO + np.arange(T)
    return np.tile(((tg >= 0) & (tg < S)).astype(np.float32)[None, :], (128, 1))


def _emit_conv_phase(nc, P, A, ps, ps_free, ident, t_params, x, ins, stop=0):
    (w1_in, b1_in, wdw_in, bdw_in, lg_in, lb_in, w2_in, b2_in, mask_in) = ins
    TB = [(0, 384), (384, 384), (768, 384)]
    identb = A.alloc([128], BF16)
    ones = A.alloc([128], F32)
    cb1 = A.alloc([32], F32)
    wdw = A.alloc([16, CONV_W], F32)
    bdw = A.alloc([16], F32)
    clg = A.alloc([16], F32)
    clb = A.alloc([16], F32)
    eps = A.alloc([1], F32)
    s_c = P.dma_sem("cs")
    for dst, src in ((cb1, b1_in), (wdw, wdw_in), (bdw, bdw_in), (clg, lg_in)):
        P.dma("sync", dst, src, s_c)
    t_sm = P.dma("sync", clb, lb_in, s_c)
    P.op("vector", lambda g: g.memset(ones, 1.0))
    P.op("vector", lambda g: g.memset(eps, LN_EPS))
    t_idb = P.op("vector", lambda g: g.tensor_copy(out=identb, in_=ident), deps=[t_params])
    R1 = A.alloc([16, T], BF16)
    R2 = A.alloc([16, T], F32)
    m_R2 = A.mark() - 16 * T * 4
    m_R3 = A.mark()
    xT, uT, vT = R1, R1, R2
    xbf = A.alloc([NT, D], BF16)
    s_x = P.dma_sem("x")
    t_xbf = P.dma("gpsimd", xbf, x.rearrange("(j p) d -> p j d", p=128), s_x)
    n = 0
    t_a = None
    for j in range(NT):
        for h in range(2):
            bk = n % 2
            n += 1
            pb = ps[bk][:, :].bitcast(BF16)
            for i in range(8):
                k = 8 * h + i
                t_tr = P.op("tensor", lambda g, j=j, k=k, i=i, pb=pb: g.transpose(
                    out=pb[:, 128 * i:128 * (i + 1)], in_=xbf[:, j, 128 * k:128 * (k + 1)], identity=identb),
                    deps=[t_xbf, t_idb, ps_free[bk]] if i == 0 else (), inc=(i == 7))
            t_a = P.op("scalar" if n % 2 else "vector", lambda g, j=j, h=h, pb=pb: (g.copy if hasattr(g, "copy") else g.tensor_copy)(
                xT[:, 8 * h:8 * (h + 1), 128 * j:128 * (j + 1)], pb.rearrange("p (a c) -> p a c", a=8)), deps=[t_tr])
            ps_free[bk] = t_a
    t_a2 = P.op("vector", lambda g: g.memset(eps, LN_EPS), deps=[t_a, (P.eng["scalar"].sem, P.eng["scalar"].count)])
    t_a_done = t_a2
    if stop == 1:
        A.reset(m_R2)
        return t_a_done, None
    A.reset(m_R3)
    NW = 2
    w1u = [A.alloc([16, 2, 128], BF16) for _ in range(NW)]
    gpad = [A.alloc([T + 2 * CONV_PAD + 2], BF16) for _ in range(2)]
    Dg = [A.alloc([CONV_W, 128], BF16) for _ in range(2)]
    sgm = [A.alloc([384], F32) for _ in range(2)]
    mask = A.alloc([T], F32)
    s_m = P.dma_sem("m")
    t_mask = P.dma("sync", mask, mask_in, s_m, deps=[t_a_done])
    t_gz = None
    for gi in range(2):
        t_gz = P.op("vector", lambda g, gi=gi: g.memset(gpad[gi], 0.0), deps=[t_a_done])
    s_w1 = [P.dma_sem("cw1") for _ in range(NW)]
    w1_free = [t_a_done] * NW
    gpad_free = [t_gz] * 2
    Dg_free = [t_a_done] * 2
    sgm_free = [t_a_done] * 2
    w1v = w1_in.rearrange("(k p) n -> p k n", p=128)
    loads = {}

    def load_w1(i):
        sl = i % NW
        P.dma("gpsimd", w1u[sl][:, :, 0, :], w1v[:, :, 128 * i:128 * (i + 1)], s_w1[sl], deps=[w1_free[sl]])
        loads[i] = P.dma("gpsimd", w1u[sl][:, :, 1, :], w1v[:, :, D + 128 * i:D + 128 * (i + 1)], s_w1[sl])

    nW = 0
    nC = 0
    nsg = 0
    state = {}

    def emit_W(i):
        nonlocal nW, nsg
        sl = i % NW
        gi = i % 2
        di = i % 2
        t_d = None
        for k in range(CONV_W):
            t_d = P.op("vector", lambda g, di=di, k=k, i=i: g.tensor_scalar(
                out=Dg[di][:, k, :], in0=identb, scalar1=wdw[:, i, k:k + 1], scalar2=None, op0=ALU.mult),
                deps=[Dg_free[di], t_sm, t_idb] if k == 0 else ())
        t_gq = None
        t_mm = None
        for (c0, cw) in TB:
            bA = 2 + 2 * (nW % 2)
            bG = bA + 1
            nW += 1
            for k in range(16):
                t_A = P.op("tensor", lambda g, k=k, sl=sl, bA=bA, c0=c0, cw=cw: g.matmul(
                    ps[bA][:, 0:cw], lhsT=w1u[sl][:, k, 0, :], rhs=xT[:, k, c0:c0 + cw], start=(k == 0), stop=(k == 15)),
                    deps=[loads[i], t_a_done, ps_free[bA]] if k == 0 else (), inc=(k == 15))
            for k in range(16):
                t_G = P.op("tensor", lambda g, k=k, sl=sl, bG=bG, c0=c0, cw=cw: g.matmul(
                    ps[bG][:, 0:cw], lhsT=w1u[sl][:, k, 1, :], rhs=xT[:, k, c0:c0 + cw], start=(k == 0), stop=(k == 15)),
                    deps=[ps_free[bG]] if k == 0 else (), inc=(k == 15))
            t_mm = t_G
            si = nsg % 2
            nsg += 1
            t1 = P.op("scalar", lambda g, si=si, bG=bG, cw=cw, i=i: g.activation(
                out=sgm[si][:, 0:cw], in_=ps[bG][:, 0:cw], func=AF.Sigmoid, bias=cb1[:, 16 + i:17 + i], scale=1.0),
                deps=[t_G, t_sm, sgm_free[si]])
            ps_free[bG] = t1
            t2 = P.op("vector", lambda g, si=si, c0=c0, cw=cw: g.tensor_tensor(
                out=sgm[si][:, 0:cw], in0=sgm[si][:, 0:cw], in1=mask[:, c0:c0 + cw], op=ALU.mult), deps=[t1, t_mask])
            t_gq = P.op("vector", lambda g, si=si, gi=gi, bA=bA, c0=c0, cw=cw, i=i: g.scalar_tensor_tensor(
                out=gpad[gi][:, CONV_PAD + c0:CONV_PAD + c0 + cw], in0=ps[bA][:, 0:cw], scalar=cb1[:, i:i + 1],
                in1=sgm[si][:, 0:cw], op0=ALU.add, op1=ALU.mult), deps=[t2, t_A, gpad_free[gi]])
            ps_free[bA] = t_gq
            sgm_free[si] = t_gq
        w1_free[sl] = t_mm
        state[i] = (t_gq, t_d)

    t_v = None

    def emit_C(i):
        nonlocal nC, t_v
        gi = i % 2
        di = i % 2
        t_gq, t_d = state[i]
        t_mm = None
        for (c0, cw) in TB:
            bV = 6 + (nC % 2)
            nC += 1
            for k in range(CONV_W):
                t_mm = P.op("tensor", lambda g, k=k, di=di, gi=gi, bV=bV, c0=c0, cw=cw: g.matmul(
                    ps[bV][:, 0:cw], lhsT=Dg[di][:, k, :], rhs=gpad[gi][:, c0 + k:c0 + k + cw],
                    start=(k == 0), stop=(k == CONV_W - 1)),
                    deps=[t_gq, t_d, ps_free[bV]] if k == 0 else (), inc=(k == CONV_W - 1))
            t_v = P.op("scalar", lambda g, bV=bV, c0=c0, cw=cw, i=i: g.activation(
                out=vT[:, i, c0:c0 + cw], in_=ps[bV][:, 0:cw], func=AF.Identity, bias=bdw[:, i:i + 1], scale=1.0),
                deps=[t_mm, t_sm])
            ps_free[bV] = t_v
        gpad_free[gi] = t_mm
        Dg_free[di] = t_mm

    load_w1(0)
    for i in range(16):
        if i + 1 < 16:
            load_w1(i + 1)
        emit_W(i)
        if i >= 1:
            emit_C(i - 1)
    emit_C(15)
    t_bc_done = t_v
    if stop == 2:
        A.reset(m_R2)
        return t_bc_done, None
    A.reset(m_R3)
    mean = A.alloc([T], F32)
    rstd = A.alloc([T], F32)
    sq = [A.alloc([384], F32) for _ in range(2)]
    tmp = [A.alloc([T], F32) for _ in range(2)]
    sq_free = [t_bc_done] * 2
    t_st = None
    nsq = 0
    for (c0, cw) in TB:
        for i in range(16):
            t_s1 = P.op("tensor", lambda g, i=i, c0=c0, cw=cw: g.matmul(
                ps[0][:, 0:cw], lhsT=ones, rhs=vT[:, i, c0:c0 + cw], start=(i == 0), stop=(i == 15)),
                deps=[t_bc_done, ps_free[0]] if i == 0 else (), inc=(i == 15))
        for i in range(16):
            qi = nsq % 2
            nsq += 1
            t_q = P.op("scalar", lambda g, i=i, qi=qi, c0=c0, cw=cw: g.activation(
                out=sq[qi][:, 0:cw], in_=vT[:, i, c0:c0 + cw], func=AF.Square), deps=[t_bc_done, sq_free[qi]])
            t_s2 = P.op("tensor", lambda g, i=i, qi=qi, cw=cw: g.matmul(
                ps[1][:, 0:cw], lhsT=ones, rhs=sq[qi][:, 0:cw], start=(i == 0), stop=(i == 15)),
                deps=[t_q, ps_free[1]] if i == 0 else [t_q], inc=True)
            sq_free[qi] = t_s2
        t = P.op("vector", lambda g, c0=c0, cw=cw: g.tensor_scalar(
            out=mean[:, c0:c0 + cw], in0=ps[0][:, 0:cw], scalar1=1.0 / D, scalar2=None, op0=ALU.mult), deps=[t_s1, t_bc_done])
        ps_free[0] = t
        t = P.op("vector", lambda g, c0=c0, cw=cw: g.tensor_tensor(
            out=rstd[:, c0:c0 + cw], in0=mean[:, c0:c0 + cw], in1=mean[:, c0:c0 + cw], op=ALU.mult), deps=[t])
        t = P.op("vector", lambda g, c0=c0, cw=cw: g.scalar_tensor_tensor(
            out=rstd[:, c0:c0 + cw], in0=ps[1][:, 0:cw], scalar=1.0 / D, in1=rstd[:, c0:c0 + cw],
            op0=ALU.mult, op1=ALU.subtract), deps=[t, t_s2])
        ps_free[1] = t
        t = P.op("scalar", lambda g, c0=c0, cw=cw: g.activation(
            out=rstd[:, c0:c0 + cw], in_=rstd[:, c0:c0 + cw], func=AF.Sqrt, bias=eps[:, 0:1], scale=1.0), deps=[t])
        t_st = P.op("vector", lambda g, c0=c0, cw=cw: g.reciprocal(out=rstd[:, c0:c0 + cw], in_=rstd[:, c0:c0 + cw]), deps=[t])
    if stop == 3:
        A.reset(m_R2)
        return t_st, None
    tmp_free = [t_bc_done] * 2
    t_u = None
    for i in range(16):
        ti = i % 2
        t = P.op("vector", lambda g, i=i, ti=ti: g.tensor_tensor(out=tmp[ti], in0=vT[:, i, :], in1=mean, op=ALU.subtract),
                 deps=[t_st, tmp_free[ti]])
        t = P.op("gpsimd", lambda g, ti=ti: g.tensor_tensor(out=tmp[ti], in0=tmp[ti], in1=rstd, op=ALU.mult), deps=[t, t_st])
        t_u = P.op("scalar", lambda g, i=i, ti=ti: g.activation(
            out=uT[:, i, :], in_=tmp[ti], func=AF.Silu, scale=clg[:, i:i + 1], bias=clb[:, i:i + 1]), deps=[t, t_sm, t_bc_done])
        tmp_free[ti] = t_u
    t_d_done = P.op("vector", lambda g: g.memset(eps, LN_EPS), deps=[t_u])
    if stop == 4:
        A.reset(m_R2)
        return t_d_done, None
    A.reset(m_R3)
    NW2, NS = 2, 2
    w2u = [A.alloc([16, 512], BF16) for _ in range(NW2)]
    cb2 = A.alloc([D], F32)
    stg = [A.alloc([512], F32) for _ in range(NS)]
    mixd = nc.dram_tensor("mixd", [T, D], F32, kind="Internal").ap()
    s_w2 = [P.dma_sem("cw2") for _ in range(NW2)]
    s_st = [P.dma_sem("cst") for _ in range(NS)]
    s_b2 = P.dma_sem("cb2")
    w2_free = [t_d_done] * NW2
    stg_free = [None] * NS
    w2v = w2_in.rearrange("(k p) n -> p k n", p=128)

    def loadw2(nb):
        sl = nb % NW2
        return P.dma("gpsimd", w2u[sl], w2v[:, :, 512 * nb:512 * (nb + 1)], s_w2[sl], deps=[w2_free[sl]])

    tl = {0: loadw2(0), 1: loadw2(1)}
    t_b2 = P.dma("gpsimd", cb2, b2_in, s_b2, deps=[t_d_done])
    n = 0
    for nb in range(4):
        sl = nb % NW2
        t_mm = None
        for j in range(NT):
            bk = 2 + (n % 4)
            si = n % NS
            n += 1
            for k in range(16):
                t_mm = P.op("tensor", lambda g, k=k, sl=sl, bk=bk, j=j: g.matmul(
                    ps[bk][:, :], lhsT=uT[:, k, 128 * j:128 * (j + 1)], rhs=w2u[sl][:, k, :], start=(k == 0), stop=(k == 15)),
                    deps=[tl[nb], t_d_done, ps_free[bk]] if k == 0 else (), inc=(k == 15))
            t_e = P.op("vector", lambda g, nb=nb, bk=bk, si=si: g.tensor_tensor(
                out=stg[si], in0=ps[bk][:, :], in1=cb2[:, 512 * nb:512 * (nb + 1)], op=ALU.add),
                deps=[t_mm, t_b2, stg_free[si]])
            ps_free[bk] = t_e
            stg_free[si] = P.dma("gpsimd", mixd[128 * j:128 * (j + 1), 512 * nb:512 * (nb + 1)], stg[si], s_st[si], deps=[t_e])
        w2_free[sl] = t_mm
        if nb + 2 < 4:
            tl[nb + 2] = loadw2(nb + 2)
    t_mix_done = [stg_free[si] for si in range(NS)]
    t_d_done = P.op("vector", lambda g: g.memset(eps, LN_EPS), deps=[t_e])
    s_mx = [P.dma_sem("mx") for _ in range(2)]

    def tail(j, mixs_b, mix_free_b):
        return P.dma("sync", mixs_b, mixd[128 * j:128 * (j + 1), :], s_mx[j % 2], deps=[mix_free_b] + t_mix_done)

    A.reset(m_R2)
    A.nbytes = m_R3
    return t_d_done, tail


def build_A(kind, stop=0):
    nc = bass.Bass("TRN2", target_bir_lowering=False)
    inp = lambda name, shape, dt=F32: nc.dram_tensor(name, shape, dt, kind="ExternalInput").ap()
    x = inp("x", [T, D])
    ident_in = inp("ident", [128, 128])
    mg_in = inp("mg_bc", [128, D])
    mb_in = inp("mb_bc", [128, D])
    wr_in = inp("wr", [D, E])
    br_in = inp("br_bc", [128, E])
    if kind == "pool":
        band_in = inp("band", [128, 4, NT * 3, 128], BF16)
        invc_in = inp("invc", [128, 4, T])
        wp_in = inp("wp", [4, 512, 512])
        sc_in = inp("sc_bc", [128, D])
    else:
        w1_in = inp("cw1", [D, 2 * D])
        b1_in = inp("cb1t", [128, 32])
        wdw_in = inp("wdwt", [128, 16, CONV_W])
        bdw_in = inp("bdwt", [128, 16])
        lg_in = inp("clgt", [128, 16])
        lb_in = inp("clbt", [128, 16])
        w2_in = inp("cw2", [D, D])
        b2_in = inp("cb2_bc", [128, D])
        mask_in = inp("mask", [128, T])
    x1o = nc.dram_tensor("x1", [T, D], F32, kind="ExternalOutput").ap()
    Go = nc.dram_tensor("G", [T, E], F32, kind="ExternalOutput").ap()
    with ExitStack() as es:
        big = es.enter_context(nc.sbuf_tensor("big", [128, SBUF_BYTES // 4], F32))
        ps = [es.enter_context(nc.psum_tensor(f"ps{i}", [128, 512], F32)) for i in range(8)]
        P = Prog(nc, es)
        A = Bump(big, SBUF_BYTES)
        ps_free = [None] * 8
        ident = A.alloc([128], F32)
        mg_bc = A.alloc([D], F32)
        mb_bc = A.alloc([D], F32)
        wr_sb = A.alloc([16, E], F32)
        br_bc = A.alloc([E], F32)
        s_p = P.dma_sem("p")
        P.dma("sync", ident, ident_in, s_p)
        P.dma("sync", mg_bc, mg_in, s_p)
        P.dma("sync", mb_bc, mb_in, s_p)
        P.dma("sync", wr_sb, wr_in.rearrange("(k p) e -> p k e", p=128), s_p)
        t_params = P.dma("sync", br_bc, br_in, s_p)
        ln = LNUnit(P, A, mg_bc, mb_bc, t_params)
        s_x = P.dma_sem("x")
        if kind == "pool":
            pooledT = A.alloc([16, T], BF16)
            wpb = A.alloc([4, 4, 512], BF16)
            sc_bc = A.alloc([D], F32)
        m_phase = A.mark()
        if kind == "pool":
            xbf = A.alloc([NT, D], BF16)
            t_xbf = P.dma("gpsimd", xbf, x.rearrange("(j p) d -> p j d", p=128), s_x)

        if kind == "pool":
            s_w = P.dma_sem("w")
            for g in range(4):
                t_wp = P.dma("gpsimd", wpb[:, g, :, :], wp_in[g].rearrange("(k p) n -> p k n", p=128), s_w)
            s_sc = P.dma_sem("sc")
            t_sc = P.dma("sync", sc_bc, sc_in, s_sc)
            band = A.alloc([4, NT * 3, 128], BF16)
            invc = A.alloc([4, T], F32)
            s_bi = P.dma_sem("bi")
            P.dma("sync", band, band_in, s_bi)
            t_bi = P.dma("sync", invc, invc_in, s_bi)
            nbank = 0
            t_ev = None
            for cc in range(16):
                g = cc // 4
                for jb in range(0, NT, 4):
                    js = list(range(jb, min(jb + 4, NT)))
                    bk = nbank % 2
                    nbank += 1
                    first = True
                    for j in js:
                        rels = [r for r in range(3) if 0 <= j + r - 1 < NT]
                        for ri, r in enumerate(rels):
                            last = (j == js[-1] and ri == len(rels) - 1)
                            t_mm = P.op("tensor", lambda gg, cc=cc, g=g, j=j, r=r, bk=bk, jb=jb, ri=ri, n=len(rels): gg.matmul(
                                ps[bk][:, 128 * (j - jb):128 * (j - jb + 1)], lhsT=xbf[:, j + r - 1, 128 * cc:128 * (cc + 1)],
                                rhs=band[:, g, 3 * j + r, :], start=(ri == 0), stop=(ri == n - 1)),
                                deps=[t_xbf, t_bi, ps_free[bk]] if first else (), inc=last)
                            first = False
                    w = 128 * len(js)
                    t_ev = P.op("vector", lambda gg, cc=cc, g=g, bk=bk, jb=jb, w=w: gg.tensor_tensor(
                        out=pooledT[:, cc, 128 * jb:128 * jb + w], in0=ps[bk][:, 0:w], in1=invc[:, g, 128 * jb:128 * jb + w],
                        op=ALU.mult), deps=[t_mm, t_bi])
                    ps_free[bk] = t_ev
            t_phase1 = t_ev
            A.reset(m_phase)
        else:
            t_phase1, conv = _emit_conv_phase(nc, P, A, ps, ps_free, ident, t_params, x,
                                              (w1_in, b1_in, wdw_in, bdw_in, lg_in, lb_in, w2_in, b2_in, mask_in), stop)

        NB = 2
        xt = [A.alloc([D], F32) for _ in range(NB)]
        mixs = [A.alloc([D], F32) for _ in range(NB)]
        x1t = [A.alloc([D], F32) for _ in range(NB)]
        x1T = [A.alloc([16, 128], F32) for _ in range(NB)]
        lg = [A.alloc([E], F32) for _ in range(NB)]
        ex = [A.alloc([E], F32) for _ in range(NB)]
        top8 = [A.alloc([8], F32) for _ in range(NB)]
        sm = [A.alloc([4], F32) for _ in range(NB)]
        Gt = [A.alloc([E], F32) for _ in range(NB)]
        s_xt = [P.dma_sem("xt") for _ in range(NB)]
        s_o = [P.dma_sem("o") for _ in range(NB)]
        s_g = [P.dma_sem("g") for _ in range(NB)]
        xt_free = [t_phase1] * NB
        mix_free = [t_phase1] * NB
        x1t_free = [[t_phase1] for _ in range(NB)]
        x1T_free = [t_phase1] * NB
        sm_free = [t_phase1] * NB
        G_free = [None] * NB
        nps = [0]
        state = {}

        def front(j):
            b = j % NB
            rows = slice(128 * j, 128 * (j + 1))
            t_xt = P.dma("sync", xt[b], x[rows, :], s_xt[b], deps=[xt_free[b]])
            yield
            if kind == "pool":
                t_m = None
                for g in range(4):
                    bk = 2 + g
                    for kk in range(4):
                        t_mm = P.op("tensor", lambda gg, g=g, kk=kk, j=j, bk=bk: gg.matmul(
                            ps[bk][:, :], lhsT=pooledT[:, 4 * g + kk, 128 * j:128 * (j + 1)], rhs=wpb[:, g, kk, :],
                            start=(kk == 0), stop=(kk == 3)),
                            deps=[t_phase1, t_wp, ps_free[bk]] if kk == 0 else (), inc=(kk == 3))
                    t_m = P.op("vector", lambda gg, g=g, b=b, bk=bk: gg.tensor_tensor(
                        out=mixs[b][:, 512 * g:512 * (g + 1)], in0=ps[bk][:, :], in1=sc_bc[:, 512 * g:512 * (g + 1)], op=ALU.mult),
                        deps=[t_mm, t_sc, mix_free[b]])
                    ps_free[bk] = t_m
                    yield
            else:
                t_m = conv(j, mixs[b], mix_free[b])
                yield
            t_z = P.op("vector", lambda gg, b=b: gg.scalar_tensor_tensor(
                out=mixs[b], in0=xt[b], scalar=ALPHA, in1=mixs[b], op0=ALU.mult, op1=ALU.add), deps=[t_m, t_xt])
            xt_free[b] = t_z
            yield
            for _ in ln.emit_gen(mixs[b], x1t[b], [t_z], o_free=x1t_free[b]):
                yield
            t_x1 = ln.last
            mix_free[b] = t_x1
            t_st = P.dma("gpsimd", x1o[rows, :], x1t[b], s_o[b], deps=[t_x1])
            state[j] = (t_x1, t_st)
            yield

        def back(j):
            b = j % NB
            rows = slice(128 * j, 128 * (j + 1))
            t_x1, t_st = state[j]
            t_cp = None
            t_tr = None
            for q in range(4):
                bk = 6 + (nps[0] % 2)
                nps[0] += 1
                for i in range(4):
                    k = 4 * q + i
                    t_tr = P.op("tensor", lambda gg, b=b, k=k, i=i, bk=bk: gg.transpose(
                        out=ps[bk][:, 128 * i:128 * (i + 1)], in_=x1t[b][:, 128 * k:128 * (k + 1)], identity=ident),
                        deps=[t_x1, t_params, ps_free[bk]] if i == 0 else (), inc=(i == 3))
                t_cp = P.op("scalar", lambda gg, b=b, q=q, bk=bk: gg.copy(
                    x1T[b][:, 4 * q:4 * (q + 1), :], ps[bk][:, :].rearrange("p (a c) -> p a c", a=4)),
                    deps=[t_tr, x1T_free[b]])
                ps_free[bk] = t_cp
                yield
            x1t_free[b] = [t_st, t_tr]
            lbk = j % 2
            t_lg = None
            for k in range(16):
                t_lg = P.op("tensor", lambda gg, b=b, k=k, lbk=lbk: gg.matmul(
                    ps[lbk][:, 0:E], lhsT=x1T[b][:, k, :], rhs=wr_sb[:, k, :], start=(k == 0), stop=(k == 15)),
                    deps=[t_cp, ps_free[lbk], t_phase1] if k == 0 else (), inc=(k == 15))
            x1T_free[b] = t_lg
            yield
            t = P.op("vector", lambda gg, b=b, lbk=lbk: gg.tensor_tensor(out=lg[b], in0=ps[lbk][:, 0:E], in1=br_bc, op=ALU.add),
                     deps=[t_lg, t_params, sm_free[b], G_free[b]])
            ps_free[lbk] = t
            yield
            t = P.op("vector", lambda gg, b=b: gg.max(out=top8[b], in_=lg[b]), deps=[t])
            yield
            t = P.op("vector", lambda gg, b=b: gg.tensor_scalar(out=sm[b][:, 0:1], in0=top8[b][:, 0:1], scalar1=-1.0, scalar2=None,
                                                                 op0=ALU.mult), deps=[t])
            yield
            t = P.op("scalar", lambda gg, b=b: gg.activation(out=ex[b], in_=lg[b], func=AF.Exp, bias=sm[b][:, 0:1], scale=1.0),
                     deps=[t])
            yield
            t = P.op("vector", lambda gg, b=b: gg.scalar_tensor_tensor(
                out=ex[b], in0=lg[b], scalar=top8[b][:, 3:4], in1=ex[b], op0=ALU.is_ge, op1=ALU.mult), deps=[t])
            yield
            t = P.op("vector", lambda gg, b=b: gg.tensor_reduce(out=sm[b][:, 1:2], in_=ex[b], axis=mybir.AxisListType.X, op=ALU.add),
                     deps=[t])
            yield
            t = P.op("vector", lambda gg, b=b: gg.reciprocal(out=sm[b][:, 2:3], in_=sm[b][:, 1:2]), deps=[t])
            yield
            t = P.op("vector", lambda gg, b=b: gg.tensor_scalar(out=Gt[b], in0=ex[b], scalar1=sm[b][:, 2:3], scalar2=None,
                                                                 op0=ALU.mult), deps=[t])
            sm_free[b] = t
            G_free[b] = P.dma("gpsimd", Go[rows, :], Gt[b], s_g[b], deps=[t])
            yield

        ntail = NT if not stop else 0
        for j in range(ntail + 1):
            gens = []
            if j < ntail:
                gens.append(front(j))
            if 1 <= j <= ntail:
                gens.append(back(j - 1))
            while gens:
                for gnr in list(gens):
                    try:
                        next(gnr)
                    except StopIteration:
                        gens.remove(gnr)
        P.wait("gpsimd", [G_free[i] for i in range(NB)] + [(s_o[i][0], s_o[i][1]) for i in range(NB) if s_o[i][1]] + [t_phase1])
        P.run()
    return nc


def _bc(v):
    return np.ascontiguousarray(np.broadcast_to(np.asarray(v, np.float32)[None, :], (128, v.shape[0])))


def _slice_halo(xfull, core):
    out = np.zeros((T, D), np.float32)
    lo = core * TOWN - HALO
    a, b = max(lo, 0), min(lo + T, S)
    out[a - lo:b - lo] = xfull[a:b]
    return out


def _chunk_t(v, n):
    return np.ascontiguousarray(np.asarray(v, np.float32).reshape(n, 128).T)


_IDENT = np.eye(128, dtype=np.float32)
_POOLC = {}


def inputs_A_pool(core, xfull, pool_w, pool_scale, mg, mb, wr, br):
    if core not in _POOLC:
        _POOLC[core] = pool_consts(core)
    band, invc = _POOLC[core]
    return {"x": _slice_halo(xfull, core), "ident": _IDENT, "mg_bc": _bc(mg), "mb_bc": _bc(mb), "wr": np.ascontiguousarray(wr),
            "br_bc": _bc(br), "band": band, "invc": invc, "wp": np.ascontiguousarray(pool_w), "sc_bc": _bc(pool_scale)}


def inputs_A_conv(core, xfull, w1, b1, wdw, bdw, lng, lnb, w2, b2, mg, mb, wr, br):
    wdwt = np.ascontiguousarray(np.asarray(wdw, np.float32).T.reshape(16, 128, CONV_W).transpose(1, 0, 2))
    return {"x": _slice_halo(xfull, core), "ident": _IDENT, "mg_bc": _bc(mg), "mb_bc": _bc(mb), "wr": np.ascontiguousarray(wr),
            "br_bc": _bc(br), "cw1": np.ascontiguousarray(w1), "cb1t": _chunk_t(b1, 32), "wdwt": wdwt,
            "bdwt": _chunk_t(bdw, 16), "clgt": _chunk_t(lng, 16), "clbt": _chunk_t(lnb, 16),
            "cw2": np.ascontiguousarray(w2), "cb2_bc": _bc(b2), "mask": valid_mask(core)}


_PROGS = {}


def _prog(key, builder):
    if key not in _PROGS:
        _PROGS[key] = builder()
    return _PROGS[key]


def _run(nc, in_maps):
    res = run_bass_kernel_spmd(nc, in_maps, core_ids=list(range(NCORES)))
    return res.results


def _lay_bias(b):
    ne = b.shape[0]
    return np.ascontiguousarray(np.asarray(b, np.float32).reshape(ne, 16, 128).transpose(2, 0, 1))


def kernel(x, pool_w, pool_scale, conv_w1, conv_b1, conv_wdw, conv_bdw, conv_ln_g, conv_ln_b, conv_w2, conv_b2,
           mix_ln_g, mix_ln_b, router_w, router_b, moe_w1, moe_b1, moe_w2, moe_b2, ffn_ln_g, ffn_ln_b):
    f32 = lambda a: np.asarray(a, dtype=np.float32)
    xcur = np.ascontiguousarray(f32(x)[0])
    cen = slice(HALO, HALO + TOWN)
    for i in range(DEPTH):
        j = i // 2
        if i % 2 == 0:
            ncA = _prog("A_pool", lambda: build_A("pool"))
            maps = [inputs_A_pool(c, xcur, f32(pool_w[j]), f32(pool_scale[j]), f32(mix_ln_g[i]), f32(mix_ln_b[i]),
                                  f32(router_w[i]), f32(router_b[i])) for c in range(NCORES)]
        else:
            ncA = _prog("A_conv", lambda: build_A("conv"))
            maps = [inputs_A_conv(c, xcur, f32(conv_w1[j]), f32(conv_b1[j]), f32(conv_wdw[j]), f32(conv_bdw[j]),
                                  f32(conv_ln_g[j]), f32(conv_ln_b[j]), f32(conv_w2[j]), f32(conv_b2[j]),
                                  f32(mix_ln_g[i]), f32(mix_ln_b[i]), f32(router_w[i]), f32(router_b[i]))
                    for c in range(NCORES)]
        res = _run(ncA, maps)
        del maps
        x1 = np.concatenate([r["x1"][cen] for r in res], axis=0)
        G = np.concatenate([r["G"][cen] for r in res], axis=0)
        del res
        sel = G > 0
        toks = [np.nonzero(sel[:, e])[0] for e in range(E)]
        Cs = tuple(max(128, (max(int(toks[c + NCORES * m].shape[0]) for c in range(NCORES)) + 127) // 128 * 128)
                   for m in range(NE))
        kpos = np.cumsum(sel, axis=1) - 1
        maps = []
        for c in range(NCORES):
            mp = {"w1": f32(moe_w1[i][c::NCORES]), "b1t": _lay_bias(f32(moe_b1[i][c::NCORES])),
                  "w2": f32(moe_w2[i][c::NCORES]), "b2t": _lay_bias(f32(moe_b2[i][c::NCORES]))}
            for m in range(NE):
                t = toks[c + NCORES * m]
                xsT = np.zeros((D, Cs[m]), np.float32)
                xsT[:, :t.shape[0]] = x1[t].T
                mp[f"xsT{m}"] = xsT
            maps.append(mp)
        ncB = _prog(("B", Cs), lambda: build_B(Cs))
        res = _run(ncB, maps)
        del maps
        yk = np.zeros((S, TOPK, D), np.float32)
        gk = np.zeros((S, TOPK), np.float32)
        for c in range(NCORES):
            for m in range(NE):
                e = c + NCORES * m
                t = toks[e]
                kp = np.minimum(kpos[t, e], TOPK - 1)
                yk[t, kp, :] = res[c][f"yT{m}"][:, :t.shape[0]].T
                gk[t, kp] = G[t, e]
        del res
        ncC = _prog("C", lambda: build_C(TOWN))
        gb, bb = _bc(f32(ffn_ln_g[i])), _bc(f32(ffn_ln_b[i]))
        maps = [{"x1": x1[c * TOWN:(c + 1) * TOWN], "yk": yk[c * TOWN:(c + 1) * TOWN], "gk": gk[c * TOWN:(c + 1) * TOWN],
                 "g_bc": gb, "b_bc": bb} for c in range(NCORES)]
        res = _run(ncC, maps)
        del maps, yk
        xcur = np.concatenate([r["x2"] for r in res], axis=0)
        del res
    return np.ascontiguousarray(xcur[None].astype(np.float32))
```
